# Optimizing a Trainium2 kernel written in Bass

```python
import math
import jax, jax.numpy as jnp
from jax import lax
import numpy as np

D_MODEL = 1024
BATCH = 16
SEQ = 2048
DEPTH = 4

HEAD_DIM = 64
NSA_Q_HEADS = 6
NSA_KV_HEADS = 2
NSA_HPG = NSA_Q_HEADS // NSA_KV_HEADS
NSA_WIDTH = NSA_Q_HEADS * HEAD_DIM
N_BRANCH = 3
NSA_KV_COLS = N_BRANCH * 2 * NSA_KV_HEADS * HEAD_DIM
CMP_BLOCK = 32
CMP_STRIDE = 16
CMP_HIDDEN = 64
SEL_BLOCK = 64
SEL_TOP_N = 16
WINDOW = 512
FORCE_SCORE = 1.0e4
GLA_HEADS = 6
GLA_DK = 32
GLA_DV = 64
GLA_WIDTH = GLA_HEADS * GLA_DV
GLA_GATE_RANK = 16
GLA_GATE_NORM = 16.0
GLA_CHUNK = 64
POOL_WINDOWS = (2, 4, 8, 16)
POOL_GROUPS = 4
POOL_GROUP = 64
POOL_WIDTH = POOL_GROUPS * POOL_GROUP
MIX_WIDTH = NSA_WIDTH + GLA_WIDTH + POOL_WIDTH
SPLIT_SIZES = (NSA_WIDTH, NSA_KV_COLS, NSA_Q_HEADS * N_BRANCH,
               GLA_HEADS * GLA_DK, GLA_HEADS * GLA_DK, GLA_WIDTH, GLA_GATE_RANK, GLA_WIDTH,
               POOL_WIDTH)
P_IN = sum(SPLIT_SIZES)
D_FF = 4 * D_MODEL
ROPE_THETA = 500000.0
ROPE_DIM = HEAD_DIM // 4
Q_BLOCK = 128
SEL_Q_BLOCK = 32
LN_EPS = 1e-5
RMS_EPS = 1e-6
DEEPNORM_ALPHA = (2 * DEPTH) ** 0.25
DEEPNORM_BETA = (8 * DEPTH) ** -0.25

kernel_name = "hymba_nsa_gla_pool_deepnorm"


def partial_rope(x, pos):
    half = ROPE_DIM // 2
    inv = ROPE_THETA ** (-jnp.arange(half, dtype=jnp.float32) * 2.0 / ROPE_DIM)
    ang = pos.astype(jnp.float32)[:, None] * inv[None, :]
    cos = jnp.cos(ang).astype(x.dtype)
    sin = jnp.sin(ang).astype(x.dtype)
    x1, x2, xp = x[..., :half], x[..., half:ROPE_DIM], x[..., ROPE_DIM:]
    return jnp.concatenate([x1 * cos - x2 * sin, x1 * sin + x2 * cos, xp], axis=-1)


def layer_norm(x, g, b):
    xf = x.astype(jnp.float32)
    mu = jnp.mean(xf, -1, keepdims=True)
    var = jnp.mean(jnp.square(xf - mu), -1, keepdims=True)
    return ((xf - mu) * lax.rsqrt(var + LN_EPS)).astype(x.dtype) * g + b


def masked_softmax(s, mask):
    s = jnp.where(mask, s.astype(jnp.float32), -jnp.inf)
    m = jnp.max(s, -1, keepdims=True)
    m = jnp.where(jnp.isfinite(m), m, 0.0)
    e = jnp.exp(s - m)
    return e / jnp.maximum(jnp.sum(e, -1, keepdims=True), 1e-30)


def nsa_mixer(q, kv, gates, cmp_pos, cmp_w1, cmp_w2):
    B, S, _ = q.shape
    G, H, Dh = NSA_KV_HEADS, NSA_HPG, HEAD_DIM
    pos = jnp.arange(S)
    q = q.reshape(B, S, G, H, Dh).transpose(0, 2, 3, 1, 4)
    q = partial_rope(q, pos) * (Dh ** -0.5)
    kv = kv.reshape(B, S, N_BRANCH, 2, G, Dh).transpose(2, 3, 0, 4, 1, 5)

    n_cmp = (S - CMP_BLOCK) // CMP_STRIDE + 1
    blk_idx = CMP_STRIDE * jnp.arange(n_cmp)[:, None] + jnp.arange(CMP_BLOCK)[None, :]

    def compress(t, p, w1, w2):
        tb = t[:, :, blk_idx] + p
        hid = jax.nn.gelu(tb.reshape(B, G, n_cmp, CMP_BLOCK * Dh) @ w1)
        return hid @ w2

    kc = compress(partial_rope(kv[0, 0], pos), cmp_pos[0], cmp_w1[0], cmp_w2[0])
    vc = compress(kv[0, 1], cmp_pos[1], cmp_w1[1], cmp_w2[1])
    s_cmp = jnp.einsum('bghsd,bgnd->bghsn', q, kc)
    cmp_mask = blk_idx[:, -1][None, :] <= pos[:, None]
    p_cmp = masked_softmax(s_cmp, cmp_mask)
    o_cmp = jnp.einsum('bghsn,bgnd->bghsd', p_cmp.astype(vc.dtype), vc)

    n_sel = S // SEL_BLOCK
    n_top = min(SEL_TOP_N, n_sel)
    c_lo = blk_idx[:, 0:1]
    s_lo = (SEL_BLOCK * jnp.arange(n_sel))[None, :]
    overlap = jnp.clip(jnp.minimum(c_lo + CMP_BLOCK, s_lo + SEL_BLOCK) - jnp.maximum(c_lo, s_lo), 0, None)
    cmp_to_sel = overlap.astype(jnp.float32) / CMP_BLOCK
    imp = jnp.einsum('bghsn,nj->bgsj', p_cmp, cmp_to_sel)
    cur = (pos // SEL_BLOCK)[:, None]
    jb = jnp.arange(n_sel)[None, :]
    imp = jnp.where((jb == 0) | (jb == cur) | (jb == cur - 1), FORCE_SCORE, imp)
    imp = jnp.where(jb > cur, -FORCE_SCORE, imp)
    _, sel_idx = lax.top_k(imp, n_top)

    k_slc = partial_rope(kv[1, 0], pos).reshape(B, G, n_sel, SEL_BLOCK, Dh)
    v_slc = kv[1, 1].reshape(B, G, n_sel, SEL_BLOCK, Dh)
    nqb = S // SEL_Q_BLOCK
    q_blk = q.reshape(B, G, H, nqb, SEL_Q_BLOCK, Dh).transpose(3, 0, 1, 2, 4, 5)
    idx_blk = sel_idx.reshape(B, G, nqb, SEL_Q_BLOCK, n_top).transpose(2, 0, 1, 3, 4)
    t_blk = pos.reshape(nqb, SEL_Q_BLOCK)
    bi = jnp.arange(B)[:, None, None, None]
    gi = jnp.arange(G)[None, :, None, None]

    def sel_block(args):
        qb, ib, tb = args
        kb = k_slc[bi, gi, ib]
        vb = v_slc[bi, gi, ib]
        s = jnp.einsum('bghqd,bgqnkd->bghqnk', qb, kb)
        kpos = ib[..., None] * SEL_BLOCK + jnp.arange(SEL_BLOCK)
        m = (kpos <= tb[None, None, :, None, None])[:, :, None]
        s = s.reshape(B, G, H, SEL_Q_BLOCK, n_top * SEL_BLOCK)
        m = m.reshape(B, G, 1, SEL_Q_BLOCK, n_top * SEL_BLOCK)
        p = masked_softmax(s, m).reshape(B, G, H, SEL_Q_BLOCK, n_top, SEL_BLOCK)
        return jnp.einsum('bghqnk,bgqnkd->bghqd', p.astype(vb.dtype), vb)

    o_slc = lax.map(sel_block, (q_blk, idx_blk, t_blk))
    o_slc = o_slc.transpose(1, 2, 3, 0, 4, 5).reshape(B, G, H, S, Dh)

    pad = ((0, 0), (0, 0), (WINDOW, 0), (0, 0))
    k_win = jnp.pad(partial_rope(kv[2, 0], pos), pad)
    v_win = jnp.pad(kv[2, 1], pad)
    nwb = S // Q_BLOCK
    span = WINDOW + Q_BLOCK
    q_wb = q.reshape(B, G, H, nwb, Q_BLOCK, Dh).transpose(3, 0, 1, 2, 4, 5)
    starts = jnp.arange(nwb) * Q_BLOCK

    def win_block(args):
        qb, s0 = args
        kb = lax.dynamic_slice_in_dim(k_win, s0, span, axis=2)
        vb = lax.dynamic_slice_in_dim(v_win, s0, span, axis=2)
        s = jnp.einsum('bghqd,bgkd->bghqk', qb, kb)
        tq = s0 + jnp.arange(Q_BLOCK)
        tk = s0 - WINDOW + jnp.arange(span)
        diff = tq[:, None] - tk[None, :]
        m = (diff >= 0) & (diff < WINDOW) & (tk[None, :] >= 0)
        p = masked_softmax(s, m)
        return jnp.einsum('bghqk,bgkd->bghqd', p.astype(vb.dtype), vb)

    o_win = lax.map(win_block, (q_wb, starts))
    o_win = o_win.transpose(1, 2, 3, 0, 4, 5).reshape(B, G, H, S, Dh)

    gt = jax.nn.sigmoid(gates.reshape(B, S, G, H, N_BRANCH).transpose(0, 2, 3, 1, 4))
    o = gt[..., 0:1] * o_cmp + gt[..., 1:2] * o_slc + gt[..., 2:3] * o_win
    return o.transpose(0, 3, 1, 2, 4).reshape(B, S, NSA_WIDTH)


def gla_mixer(q, k, v, g_lr, og, w_gate2, b_gate, norm_g):
    B, S, _ = q.shape
    Hh, dk, dv, C = GLA_HEADS, GLA_DK, GLA_DV, GLA_CHUNK
    N = S // C
    f32 = jnp.float32
    gk = jax.nn.log_sigmoid((g_lr @ w_gate2 + b_gate).astype(f32)) / GLA_GATE_NORM

    def chunks(t, d):
        return t.astype(f32).reshape(B, N, C, Hh, d).transpose(0, 3, 1, 2, 4)

    qc = chunks(q, dk) * (dk ** -0.5)
    kc = chunks(k, dk)
    vc = chunks(v, dv)
    bcum = jnp.cumsum(chunks(gk, dk), axis=3)
    blast = bcum[:, :, :, -1:]
    q_dec = qc * jnp.exp(bcum)
    k_inv = kc * jnp.exp(-bcum)
    k_end = kc * jnp.exp(blast - bcum)
    causal = jnp.tril(jnp.ones((C, C), bool))
    a = jnp.where(causal, jnp.einsum('bhnid,bhnjd->bhnij', q_dec, k_inv), 0.0)
    o_intra = jnp.einsum('bhnij,bhnjv->bhniv', a, vc)
    d_state = jnp.einsum('bhncd,bhncv->bhndv', k_end, vc)
    decay = jnp.exp(blast[:, :, :, 0])

    def step(s_prev, inp):
        dcy, ds = inp
        return s_prev * dcy[..., None] + ds, s_prev

    _, s_in = lax.scan(step, jnp.zeros((B, Hh, dk, dv), f32),
                       (decay.transpose(2, 0, 1, 3), d_state.transpose(2, 0, 1, 3, 4)))
    s_in = s_in.transpose(1, 2, 0, 3, 4)
    o = o_intra + jnp.einsum('bhncd,bhndv->bhncv', q_dec, s_in)
    o = o * lax.rsqrt(jnp.mean(o * o, -1, keepdims=True) + RMS_EPS)
    o = o.transpose(0, 2, 3, 1, 4).reshape(B, S, GLA_WIDTH).astype(og.dtype)
    return o * norm_g * jax.nn.silu(og)


def pool_mixer(u, w_pool, scale):
    B, S, Cw = u.shape
    uf = u.astype(jnp.float32)
    cs = jnp.pad(jnp.cumsum(uf, axis=1), ((0, 0), (1, 0), (0, 0)))
    win = jnp.repeat(jnp.array(POOL_WINDOWS, jnp.int32), POOL_GROUP)
    t = jnp.arange(S)[:, None]
    lo = jnp.maximum(t + 1 - win[None, :], 0)
    cnt = jnp.minimum(t + 1, win[None, :]).astype(jnp.float32)
    lo_sum = jnp.take_along_axis(cs, jnp.broadcast_to(lo[None], (B, S, Cw)), axis=1)
    pooled = (cs[:, 1:] - lo_sum) / cnt - uf
    y = jnp.einsum('bsgc,gcd->bsgd', pooled.reshape(B, S, POOL_GROUPS, POOL_GROUP).astype(u.dtype), w_pool)
    return y.reshape(B, S, POOL_WIDTH) * scale


def setup_inputs(seed: int = 0) -> dict:
    key = jax.random.key(seed)
    ks = jax.random.split(key, 20)
    L, D = DEPTH, D_MODEL
    nrm = lambda k, shape, s: jax.random.normal(k, shape, jnp.float32) * s
    return {
        "x": nrm(ks[0], (BATCH, SEQ, D), 1.0),
        "w_in": nrm(ks[1], (L, D, P_IN), D ** -0.5),
        "cmp_pos": nrm(ks[2], (L, 2, CMP_BLOCK, HEAD_DIM), 0.1),
        "cmp_w1": nrm(ks[3], (L, 2, CMP_BLOCK * HEAD_DIM, CMP_HIDDEN), (CMP_BLOCK * HEAD_DIM) ** -0.5),
        "cmp_w2": nrm(ks[4], (L, 2, CMP_HIDDEN, HEAD_DIM), CMP_HIDDEN ** -0.5),
        "gla_w_gate2": nrm(ks[5], (L, GLA_GATE_RANK, GLA_HEADS * GLA_DK), GLA_GATE_RANK ** -0.5),
        "gla_b_gate": nrm(ks[6], (L, GLA_HEADS * GLA_DK), 0.01),
        "gla_norm_g": 1.0 + nrm(ks[7], (L, GLA_WIDTH), 0.02),
        "pool_w": nrm(ks[8], (L, POOL_GROUPS, POOL_GROUP, POOL_GROUP), POOL_GROUP ** -0.5),
        "pool_scale": 1.0 + nrm(ks[9], (L, POOL_WIDTH), 0.02),
        "w_out": nrm(ks[10], (L, MIX_WIDTH, D), MIX_WIDTH ** -0.5 * DEEPNORM_BETA),
        "ln1_g": 1.0 + nrm(ks[11], (L, D), 0.02),
        "ln1_b": nrm(ks[12], (L, D), 0.02),
        "w_up": nrm(ks[13], (L, D, D_FF), D ** -0.5),
        "w_down": nrm(ks[14], (L, D_FF, D), D_FF ** -0.5 * DEEPNORM_BETA),
        "ln2_g": 1.0 + nrm(ks[15], (L, D), 0.02),
        "ln2_b": nrm(ks[16], (L, D), 0.02),
    }


def reference(x, w_in, cmp_pos, cmp_w1, cmp_w2, gla_w_gate2, gla_b_gate, gla_norm_g, pool_w, pool_scale,
              w_out, ln1_g, ln1_b, w_up, w_down, ln2_g, ln2_b):
    split_points = []
    acc = 0
    for sz in SPLIT_SIZES[:-1]:
        acc += sz
        split_points.append(acc)
    h = x
    for l in range(DEPTH):
        proj = h @ w_in[l]
        nq, nkv, ngate, gq, gk, gv, glr, gog, pu = jnp.split(proj, split_points, axis=-1)
        mixed = jnp.concatenate([
            nsa_mixer(nq, nkv, ngate, cmp_pos[l], cmp_w1[l], cmp_w2[l]),
            gla_mixer(gq, gk, gv, glr, gog, gla_w_gate2[l], gla_b_gate[l], gla_norm_g[l]),
            pool_mixer(pu, pool_w[l], pool_scale[l]),
        ], axis=-1)
        h = layer_norm(DEEPNORM_ALPHA * h + mixed @ w_out[l], ln1_g[l], ln1_b[l])
        ff = jnp.square(jax.nn.relu(h @ w_up[l])) @ w_down[l]
        h = layer_norm(DEEPNORM_ALPHA * h + ff, ln2_g[l], ln2_b[l])
    return h
```

```python
import math
import numpy as np
from contextlib import ExitStack
import concourse.bass as bass
import concourse.mybir as mybir
from concourse.bass_utils import run_bass_kernel_spmd
from concourse.ap import AP

F32 = mybir.dt.float32
BF16 = mybir.dt.bfloat16
AF = mybir.ActivationFunctionType
ALU = mybir.AluOpType
AX = mybir.AxisListType

SEQ = 2048
NT = 16
D = 1024
KC = 8
DEPTH = 4
P_IN = 2594
D_FF = 4096
ALPHA = (2 * DEPTH) ** 0.25
LN_EPS = 1e-5
RMS_EPS = 1e-6
NEG = -30000.0
NCMP = 127


class Tile:
    __slots__ = ("ap", "lw", "rd", "dsem", "dcnt", "name", "ps")

    def __init__(self, ap, name="", ps=False):
        self.ap = ap
        self.ps = ps
        self.lw = None
        self.rd = {}
        self.dsem = None
        self.dcnt = 0
        self.name = name

    def __getitem__(self, k):
        return self.ap[k]


class Sched:
    def __init__(self, nc, es):
        self.nc = nc
        self.es = es
        self.eng = {"pe": nc.tensor, "act": nc.scalar, "dve": nc.vector, "pool": nc.gpsimd, "sp": nc.sync}
        self.sem = {}
        self.cnt = {}
        for e in ("pe", "act", "dve", "pool"):
            self.sem[e] = es.enter_context(nc.semaphore("sem_" + e))
            self.cnt[e] = 0
        self.waited = {e: {} for e in self.eng}
        self.nops = 0
        self.nwaits = 0

    def attach_dma_sem(self, tile, name=None):
        name = name or ("d_" + tile.name)
        if name not in self.sem:
            self.sem[name] = self.es.enter_context(self.nc.semaphore(name))
        tile.dsem = name
        return tile

    def share_dma_sem(self, tile, other):
        tile.dsem = other.dsem
        return tile

    def _sync(self, eng, reads, writes):
        needs = {}

        def need(k, v):
            if eng == "pe" and k == "pe":
                return
            if needs.get(k, 0) < v:
                needs[k] = v

        for t in reads:
            if t.lw is not None:
                need(*t.lw)
            if t.ps:
                for k, v in t.rd.items():
                    if k != eng:
                        need(k, v)
        for t in writes:
            if t.lw is not None and t.lw[0] != eng:
                need(*t.lw)
            for k, v in t.rd.items():
                if k != eng:
                    need(k, v)
        w = self.waited[eng]
        for k, v in needs.items():
            if w.get(k, 0) < v:
                self.eng[eng].wait_ge(self.sem[k], v)
                w[k] = v
                self.nwaits += 1

    max_ops = None

    def op(self, eng, reads, writes, fn, inc=True):
        if self.max_ops is not None and self.nops >= self.max_ops:
            return None
        self._sync(eng, reads, writes)
        ins = fn()
        self.nops += 1
        if inc:
            self.cnt[eng] += 1
            ins.then_inc(self.sem[eng], 1)
            val = self.cnt[eng]
        else:
            val = self.cnt[eng] + 1
        for t in reads:
            if t.rd.get(eng, 0) < val:
                t.rd[eng] = val
        for t in writes:
            t.lw = (eng, val)
            t.rd = {}
        return ins

    def dma(self, q, out_t, in_t, out_ap, in_ap, **kw):
        self._sync(q, [in_t], [out_t])
        ins = self.eng[q].dma_start(out=out_ap, in_=in_ap, **kw)
        key = out_t.dsem
        base = self._dcnt.get(key, 0) + 16
        self._dcnt[key] = base
        ins.then_inc(self.sem[key], 16)
        self.nops += 1
        in_t.rd[key] = base
        out_t.lw = (key, base)
        out_t.rd = {}
        return ins

    _dcnt = None

    def wait_tile(self, eng, t):
        self._sync(eng, [t], [])

    def barrier(self):
        for e in ("pe", "act", "dve", "pool", "sp"):
            w = self.waited[e]
            for f in ("pe", "act", "dve", "pool"):
                if f != e and w.get(f, 0) < self.cnt[f]:
                    self.eng[e].wait_ge(self.sem[f], self.cnt[f])
                    w[f] = self.cnt[f]


def _consts():
    c = {}
    c["ident"] = np.eye(128, dtype=np.float32)
    pos = np.arange(SEQ, dtype=np.float32)
    half = 8
    inv = (np.float32(500000.0) ** (-np.arange(half, dtype=np.float32) * np.float32(2.0) / np.float32(16))).astype(np.float32)
    ang = (pos[:, None] * inv[None, :]).astype(np.float32)
    cos = np.cos(ang).astype(np.float32)
    sin = np.sin(ang).astype(np.float32)
    rope = np.zeros((128, 4, NT, 8), np.float32)
    cs = cos.reshape(NT, 128, 8).transpose(1, 0, 2)
    sn = sin.reshape(NT, 128, 8).transpose(1, 0, 2)
    rope[:, 0] = cs * 0.125
    rope[:, 1] = sn * 0.125
    rope[:, 2] = cs
    rope[:, 3] = sn
    c["rope"] = rope
    n = np.arange(128)[:, None]
    q = np.arange(SEQ)[None, :]
    c["cmpmask"] = ((16 * n + 31) <= q).astype(np.float32)
    k = np.arange(128)[:, None]
    qq = np.arange(128)[None, :]
    tri = np.zeros((128, 2, 128), np.float32)
    tri[:, 0] = np.where(k <= qq, 0.0, NEG)
    tri[:, 1] = np.where(k > qq, 0.0, NEG)
    c["tri"] = tri
    c["efull"] = (np.arange(32)[:, None] == (np.arange(SEQ)[None, :] // 64)).astype(np.float32)
    t_idx = np.arange(SEQ)
    cur = t_idx // 64
    jb = np.arange(32)[None, :]
    A = np.ones((SEQ, 32), np.float32)
    Bm = np.zeros((SEQ, 32), np.float32)
    forced0 = (jb == 0)
    forcedc = (jb == cur[:, None])
    forcedp = (jb == cur[:, None] - 1)
    fut = jb > cur[:, None]
    A[np.broadcast_to(forced0, A.shape) | forcedc | forcedp | fut] = 0.0
    Bm = np.where(np.broadcast_to(forced0, A.shape), 8192.0, Bm)
    Bm = np.where(forcedp, 8256.0, Bm)
    Bm = np.where(forcedc, 8320.0, Bm)
    Bm = np.where(fut, -8192.0 - 64.0 * jb, Bm).astype(np.float32)
    frc = np.zeros((128, 2, NT, 32), np.float32)
    frc[:, 0] = A.reshape(NT, 128, 32).transpose(1, 0, 2)
    frc[:, 1] = Bm.reshape(NT, 128, 32).transpose(1, 0, 2)
    c["force"] = frc
    nn = np.arange(128)
    c_lo = 16 * nn[:, None]
    s_lo = 64 * np.arange(32)[None, :]
    ov = np.clip(np.minimum(c_lo + 32, s_lo + 64) - np.maximum(c_lo, s_lo), 0, None)
    c["c2s"] = (ov.astype(np.float32) / 32.0)
    tp = np.arange(128)[:, None]
    tt = np.arange(128)[None, :]
    gl = np.zeros((128, 3, 128), np.float32)
    gl[:, 0] = (tp <= tt)
    gl[:, 1] = (tp > tt)
    gl[:, 2] = (tt >= tp)
    c["glamat"] = gl
    band = np.zeros((128, 3, 4, 128), np.float32)
    for gi, w in enumerate((2, 4, 8, 16)):
        for tok in range(128):
            cnt0 = min(tok + 1, w)
            for t_ in range(max(0, tok - w + 1), tok + 1):
                band[t_, 0, gi, tok] += 1.0 / cnt0
            band[tok, 0, gi, tok] -= 1.0
            for d_ in range(w):
                src = tok - d_
                if src >= 0:
                    band[src, 1, gi, tok] += 1.0 / w
                else:
                    band[128 + src, 2, gi, tok] += 1.0 / w
            band[tok, 1, gi, tok] -= 1.0
    c["band"] = band
    cv = np.zeros((128, 8), np.float32)
    cv[:, 0] = LN_EPS
    cv[:, 1] = RMS_EPS
    cv[:, 2] = 1.0
    c["cvec"] = cv
    return c


CONST_SHAPES = {
    "ident": [128, 128], "rope": [128, 4, NT, 8], "cmpmask": [128, SEQ], "tri": [128, 2, 128],
    "efull": [32, SEQ], "force": [128, 2, NT, 32], "c2s": [128, 32], "glamat": [128, 3, 128],
    "band": [128, 3, 4, 128], "cvec": [128, 8],
}

WEIGHT_SHAPES = {
    "w_in": [DEPTH, D, P_IN], "cmp_pos": [DEPTH, 2, 32, 64], "cmp_w1": [DEPTH, 2, 2048, 64],
    "cmp_w2": [DEPTH, 2, 64, 64], "gla_w_gate2": [DEPTH, 16, 192], "gla_b_gate": [DEPTH, 192],
    "gla_norm_g": [DEPTH, 384], "pool_w": [DEPTH, 4, 64, 64], "pool_scale": [DEPTH, 256],
    "w_out": [DEPTH, D, D], "ln1_g": [DEPTH, D], "ln1_b": [DEPTH, D], "w_up": [DEPTH, D, D_FF],
    "w_down": [DEPTH, D_FF, D], "ln2_g": [DEPTH, D], "ln2_b": [DEPTH, D],
}


class Builder:
    def __init__(self, n_seq=2, layers=(0, 1, 2, 3), phases=("nsa", "gla", "pool", "out", "ffn"), dump=None):
        self.n_seq = n_seq
        self.layers = list(layers)
        self.phases = phases
        self.dump = dump
        nc = self.nc = bass.Bass("TRN2", target_bir_lowering=False)
        es = self.es = ExitStack()
        S = self.S = Sched(nc, es)
        S._dcnt = {}
        self.x_d = Tile(nc.dram_tensor("x", [n_seq, SEQ, D], F32, kind="ExternalInput").ap(), "x")
        self.out_d = [S.attach_dma_sem(Tile(nc.dram_tensor("out", [n_seq, SEQ, D], F32, kind="ExternalOutput").ap(), "out"))]
        self.w = {k: Tile(nc.dram_tensor(k, shp, F32, kind="ExternalInput").ap(), k) for k, shp in WEIGHT_SHAPES.items()}
        self.c = {k: Tile(nc.dram_tensor("c_" + k, shp, F32, kind="ExternalInput").ap(), "c_" + k) for k, shp in CONST_SHAPES.items()}
        self.dump_d = {}
        self.h = self.sb("h", [128, NT, D], F32)
        self.h_t = [S.attach_dma_sem(Tile(self.h[:, t, :], f"h{t}")) for t in range(NT)]
        self.hT = self.sb("hT", [128, KC, SEQ], BF16)
        self.hT_t = [Tile(self.hT[:, :, t * 128:(t + 1) * 128], f"hT{t}") for t in range(NT)]
        self.mT = self.sb("mT", [128, KC, SEQ], BF16)
        self.mT_t = [Tile(self.mT[:, :, t * 128:(t + 1) * 128], f"mT{t}") for t in range(NT)]
        self.wbuf = [S.attach_dma_sem(Tile(self.sb(f"wb{i}", [128, KC, 512], BF16), f"wb{i}")) for i in range(3)]
        self.wrr = 0
        self.identb = S.attach_dma_sem(Tile(self.sb("identb", [128, 128], BF16), "identb"))
        self.cvec = S.attach_dma_sem(Tile(self.sb("cvec", [128, 8], F32), "cvec"))
        self.PS = es.enter_context(nc.psum_tensor("PS", [128, 4096], F32))
        self.bank = [Tile(self.PS[:, i * 512:(i + 1) * 512], f"bank{i}", ps=True) for i in range(8)]
        self.brr = 0
        S.dma("pool", self.identb, self.c["ident"], self.identb.ap, self.c["ident"].ap)
        S.dma("sp", self.cvec, self.c["cvec"], self.cvec.ap, self.c["cvec"].ap)

    _uid = 0

    def sb(self, name, shape, dt, es=None):
        self._uid += 1
        return (es or self.es).enter_context(self.nc.sbuf_tensor(f"{name}_{self._uid}", shape, dt))[:]

    def sbt(self, name, shape, dt, es=None, dma=False):
        t = Tile(self.sb(name, shape, dt, es), name)
        if dma:
            self.S.attach_dma_sem(t, "d_" + name)
        return t

    marks = None

    def mark(self, name):
        if self.marks is not None:
            self.marks.append((name, self.S.nops))

    def next_bank(self):
        b = self.bank[self.brr % 8]
        self.brr += 1
        return b

    def load_w(self, src_tile, src_ap, ncols):
        wb = self.wbuf[self.wrr % 3]
        self.wrr += 1
        self.S.dma("pool", wb, src_tile, wb.ap[:, :, 0:ncols], src_ap)
        return wb

    def dump_sb(self, name, tile, ap, shape, dt=F32):
        if self.dump is None or name not in self.dump:
            return
        S = self.S
        if name not in self.dump_d:
            self.dump_d[name] = S.attach_dma_sem(Tile(self.nc.dram_tensor("dbg_" + name, shape, dt, kind="ExternalOutput").ap(), "dbg_" + name))
        return self.dump_d[name]

    def build(self):
        S = self.S
        for s in range(self.n_seq):
            for t in range(NT):
                S.dma("sp", self.h_t[t], self.x_d, self.h_t[t].ap, self.x_d.ap[s, t * 128:(t + 1) * 128, :])
            with ExitStack() as es0:
                self.alloc_hb(es0)
                for t in range(NT):
                    self.make_hT(t)
                S.barrier()
            for l in self.layers:
                self.layer(l, s)
            if self.dump and "mT" in self.dump and s == 0:
                dd = S.attach_dma_sem(Tile(self.nc.dram_tensor("dbg_mT", [128, KC, SEQ], BF16, kind="ExternalOutput").ap(), "dbg_mT"))
                self.dump_d["mT"] = dd
                allm = Tile(self.mT, "mT_all")
                S.barrier()
                S.dma("sp", dd, allm, dd.ap, self.mT)
            for t in range(NT):
                S.dma("sp", self.out_d[0], self.h_t[t], self.out_d[0].ap[s, t * 128:(t + 1) * 128, :], self.h_t[t].ap)
        S.wait_tile("sp", self.out_d[0])
        for dd in self.dump_d.values():
            S.wait_tile("sp", dd)
        S.barrier()
        self.es.close()
        return self.nc

    def alloc_hb(self, es):
        self._hb = [Tile(self.sb(f"hb{i}", [128, D], BF16, es), f"hb{i}") for i in range(2)]
        self._ln = [dict(st=Tile(self.sb(f"lnst{i}", [128, 2, 6], F32, es)), mv=Tile(self.sb(f"lnmv{i}", [128, 2], F32, es)),
                         rs=Tile(self.sb(f"lnrs{i}", [128, 2], F32, es))) for i in range(2)]

    def make_hT(self, t):
        S, nc = self.S, self.nc
        hb = self._hb[t % 2]
        ht = self.h_t[t]
        S.op("act", [ht], [hb], lambda: nc.scalar.copy(hb.ap, ht.ap))
        bk = self.next_bank()
        pb = bk.ap.bitcast(BF16)
        for kc in range(KC):
            S.op("pe", [hb, self.identb], [bk],
                 lambda kc=kc: nc.tensor.transpose(pb[:, kc * 128:(kc + 1) * 128], hb.ap[:, kc * 128:(kc + 1) * 128], self.identb.ap),
                 inc=(kc == KC - 1))
        dst = self.hT_t[t]
        S.op("dve", [bk], [dst], lambda: nc.vector.tensor_copy(dst.ap, pb.rearrange("p (k c) -> p k c", k=KC)))

    def layer(self, l, s):
        if "nsa" in self.phases:
            self.nsa_phase(l)
        if "gla" in self.phases:
            self.gla_phase(l)
        if "pool" in self.phases:
            self.pool_phase(l)
        if "out" in self.phases:
            self.outproj_ln1(l)
        if "ffn" in self.phases:
            self.ffn_ln2(l)

    def ln_tile(self, t, tabs):
        S, nc = self.S, self.nc
        ht = self.h_t[t]
        b = self._ln[t % 2]
        st, mv, rs = b["st"], b["mv"], b["rs"]
        S.op("dve", [ht], [st], lambda: nc.vector.bn_stats(st.ap[:, 0, :], ht.ap[:, 0:512]))
        S.op("dve", [ht], [st], lambda: nc.vector.bn_stats(st.ap[:, 1, :], ht.ap[:, 512:1024]))
        S.op("dve", [st], [mv], lambda: nc.vector.bn_aggr(mv.ap, st.ap))
        S.op("act", [mv, self.cvec], [rs], lambda: nc.scalar.activation(out=rs.ap[:, 0:1], in_=mv.ap[:, 1:2], func=AF.Sqrt, bias=self.cvec.ap[:, 0:1], scale=1.0))
        S.op("dve", [rs], [rs], lambda: nc.vector.reciprocal(rs.ap[:, 0:1], rs.ap[:, 0:1]))
        S.op("dve", [rs, mv], [rs], lambda: nc.vector.scalar_tensor_tensor(out=rs.ap[:, 1:2], in0=mv.ap[:, 0:1], scalar=-1.0, in1=rs.ap[:, 0:1], op0=ALU.mult, op1=ALU.mult))
        S.op("act", [ht, rs], [ht], lambda: nc.scalar.activation(out=ht.ap, in_=ht.ap, func=AF.Identity, scale=rs.ap[:, 0:1], bias=rs.ap[:, 1:2]))
        S.op("dve", [ht, tabs[0]], [ht], lambda: nc.vector.tensor_tensor(out=ht.ap, in0=ht.ap, in1=tabs[0].ap, op=ALU.mult))
        S.op("dve", [ht, tabs[1]], [ht], lambda: nc.vector.tensor_tensor(out=ht.ap, in0=ht.ap, in1=tabs[1].ap, op=ALU.add))
        self.make_hT(t)

    def load_ln_tabs(self, es, gname, bname, l):
        S = self.S
        G = self.sbt("lnG", [128, D], F32, es, dma=True)
        Bt = self.sbt("lnB", [128, D], F32, es, dma=True)
        S.dma("sp", G, self.w[gname], G.ap, self.w[gname].ap[l:l + 1, :].partition_broadcast(128))
        S.dma("sp", Bt, self.w[bname], Bt.ap, self.w[bname].ap[l:l + 1, :].partition_broadcast(128))
        return G, Bt

    def outproj_ln1(self, l):
        S, nc = self.S, self.nc
        with ExitStack() as es:
            self.alloc_hb(es)
            tabs = self.load_ln_tabs(es, "ln1_g", "ln1_b", l)
            wo = self.w["w_out"]
            wts = [self.load_w(wo, wo.ap[l].rearrange("(k p) c -> p k c", p=128)[:, :, hf * 512:(hf + 1) * 512], 512) for hf in range(2)]
            for t in range(NT):
                ht = self.h_t[t]
                for hf in range(2):
                    bk = self.next_bank()
                    for kc in range(KC):
                        S.op("pe", [self.mT_t[t], wts[hf]], [bk],
                             lambda kc=kc, bk=bk, hf=hf: nc.tensor.matmul(bk.ap, self.mT[:, kc, t * 128:(t + 1) * 128], wts[hf].ap[:, kc, :], start=(kc == 0), stop=(kc == KC - 1)),
                             inc=(kc == KC - 1))
                    S.op("dve", [ht, bk], [ht],
                         lambda bk=bk, hf=hf: nc.vector.scalar_tensor_tensor(out=ht.ap[:, hf * 512:(hf + 1) * 512], in0=ht.ap[:, hf * 512:(hf + 1) * 512], scalar=ALPHA, in1=bk.ap, op0=ALU.mult, op1=ALU.add))
                self.ln_tile(t, tabs)
            S.barrier()

    def ffn_ln2(self, l):
        S, nc = self.S, self.nc
        with ExitStack() as es:
            self.alloc_hb(es)
            tabs = self.load_ln_tabs(es, "ln2_g", "ln2_b", l)
            hid = self.sb("hidT", [128, KC, SEQ], BF16, es)
            hid_c = [[Tile(hid[:, c, tg * 512:(tg + 1) * 512], f"hid{c}_{tg}") for tg in range(4)] for c in range(KC)]
            rtmp = [Tile(self.sb(f"rtmp{i}", [128, 512], BF16, es), f"rtmp{i}") for i in range(3)]
            wu, wd = self.w["w_up"], self.w["w_down"]
            rr = 0
            for fg in range(4):
                for wt in range(2):
                    f0 = fg * 1024 + wt * 512
                    Wt = self.load_w(wu, wu.ap[l].rearrange("(k p) c -> p k c", p=128)[:, :, f0:f0 + 512], 512)
                    for fc in range(4):
                        c = wt * 4 + fc
                        for tg in range(4):
                            bk = self.next_bank()
                            for kc in range(KC):
                                S.op("pe", [self.hT_t[4 * tg + i] for i in range(4)] + [Wt], [bk],
                                     lambda kc=kc, bk=bk, fc=fc, tg=tg, Wt=Wt: nc.tensor.matmul(bk.ap, Wt.ap[:, kc, fc * 128:(fc + 1) * 128], self.hT[:, kc, tg * 512:(tg + 1) * 512], start=(kc == 0), stop=(kc == KC - 1)),
                                     inc=(kc == KC - 1))
                            rt = rtmp[rr % 3]
                            rr += 1
                            S.op("act", [bk], [rt], lambda bk=bk, rt=rt: nc.scalar.activation(out=rt.ap, in_=bk.ap, func=AF.Relu))
                            hc = hid_c[c][tg]
                            S.op("dve", [bk, rt], [hc], lambda bk=bk, rt=rt, hc=hc: nc.vector.tensor_tensor(out=hc.ap, in0=bk.ap, in1=rt.ap, op=ALU.mult))
                for hf in range(2):
                    Wt = self.load_w(wd, wd.ap[l, fg * 1024:(fg + 1) * 1024, :].rearrange("(k p) c -> p k c", p=128)[:, :, hf * 512:(hf + 1) * 512], 512)
                    for t in range(NT):
                        ht = self.h_t[t]
                        bk = self.next_bank()
                        for kc in range(KC):
                            S.op("pe", [hid_c[kc][t // 4], Wt], [bk],
                                 lambda kc=kc, bk=bk, t=t, Wt=Wt: nc.tensor.matmul(bk.ap, hid[:, kc, t * 128:(t + 1) * 128], Wt.ap[:, kc, :], start=(kc == 0), stop=(kc == KC - 1)),
                                 inc=(kc == KC - 1))
                        sl = slice(hf * 512, (hf + 1) * 512)
                        if fg == 0:
                            S.op("dve", [ht, bk], [ht], lambda bk=bk, ht=ht, sl=sl: nc.vector.scalar_tensor_tensor(out=ht.ap[:, sl], in0=ht.ap[:, sl], scalar=ALPHA, in1=bk.ap, op0=ALU.mult, op1=ALU.add))
                        else:
                            S.op("dve", [ht, bk], [ht], lambda bk=bk, ht=ht, sl=sl: nc.vector.tensor_tensor(out=ht.ap[:, sl], in0=ht.ap[:, sl], in1=bk.ap, op=ALU.add))
                        if fg == 3 and hf == 1:
                            self.ln_tile(t, tabs)
            S.barrier()

    def pool_phase(self, l):
        S, nc = self.S, self.nc
        with ExitStack() as es:
            band = self.sbt("band", [128, 3, 4, 128], BF16, es, dma=True)
            S.dma("pool", band, self.c["band"], band.ap, self.c["band"].ap)
            wpb = self.sbt("wpb", [128, 2, 128], BF16, es, dma=True)
            S.op("dve", [], [wpb], lambda: nc.vector.memset(wpb.ap, 0.0))
            pw = self.w["pool_w"]
            for g in range(4):
                r0 = (g % 2) * 64
                S.dma("pool", wpb, pw, wpb.ap[r0:r0 + 64, g // 2, r0:r0 + 64], pw.ap[l, g])
            psc = self.sbt("psc", [128, 2], F32, es, dma=True)
            S.dma("sp", psc, self.w["pool_scale"], psc.ap, self.w["pool_scale"].ap[l].rearrange("(a p) -> p a", p=128), allow_slow_non_contiguous=True)
            u = [Tile(self.sb(f"pu{i}", [128, 256], BF16, es), f"pu{i}") for i in range(2)]
            pT = [Tile(self.sb(f"pT{i}", [128, 2, 128], BF16, es), f"pT{i}") for i in range(2)]
            wi = self.w["w_in"]
            Wt = self.load_w(wi, wi.ap[l].rearrange("(k p) c -> p k c", p=128)[:, :, 2338:2594], 256)
            for t in range(NT):
                bk = self.next_bank()
                for kc in range(KC):
                    S.op("pe", [self.hT_t[t], Wt], [bk],
                         lambda kc=kc, bk=bk: nc.tensor.matmul(bk.ap[:, 0:256], self.hT[:, kc, t * 128:(t + 1) * 128], Wt.ap[:, kc, 0:256], start=(kc == 0), stop=(kc == KC - 1)),
                         inc=(kc == KC - 1))
                uc, up = u[t % 2], u[(t + 1) % 2]
                S.op("act", [bk], [uc], lambda bk=bk, uc=uc: nc.scalar.copy(uc.ap, bk.ap[:, 0:256]))
                b2 = self.next_bank()
                for g in range(4):
                    r0 = (g % 2) * 64
                    o = b2.ap[r0:r0 + 64, (g // 2) * 128:(g // 2 + 1) * 128]
                    if t == 0:
                        S.op("pe", [uc, band], [b2], lambda o=o, g=g, uc=uc: nc.tensor.matmul(o, uc.ap[:, g * 64:(g + 1) * 64], band.ap[:, 0, g, :], start=True, stop=True), inc=(g == 3))
                    else:
                        S.op("pe", [uc, band], [b2], lambda o=o, g=g, uc=uc: nc.tensor.matmul(o, uc.ap[:, g * 64:(g + 1) * 64], band.ap[:, 1, g, :], start=True, stop=False), inc=False)
                        S.op("pe", [up, band], [b2], lambda o=o, g=g, up=up: nc.tensor.matmul(o, up.ap[:, g * 64:(g + 1) * 64], band.ap[:, 2, g, :], start=False, stop=True), inc=(g == 3))
                pt = pT[t % 2]
                S.op("dve", [b2], [pt], lambda b2=b2, pt=pt: nc.vector.tensor_copy(pt.ap, b2.ap[:, 0:256].rearrange("p (a c) -> p a c", a=2)))
                b3 = self.next_bank()
                for a in range(2):
                    S.op("pe", [pt, wpb], [b3], lambda a=a, b3=b3, pt=pt: nc.tensor.matmul(b3.ap[:, a * 128:(a + 1) * 128], wpb.ap[:, a, :], pt.ap[:, a, :], start=True, stop=True), inc=(a == 1))
                for a in range(2):
                    S.op("act", [b3, psc], [self.mT_t[t]], lambda a=a, b3=b3: nc.scalar.activation(out=self.mT[:, 6 + a, t * 128:(t + 1) * 128], in_=b3.ap[:, a * 128:(a + 1) * 128], func=AF.Copy, scale=psc.ap[:, a:a + 1]))
            S.barrier()

    def gla_phase(self, l):
        S, nc = self.S, self.nc
        with ExitStack() as es:
            gm = self.sbt("glamat", [128, 3, 128], BF16, es, dma=True)
            S.dma("pool", gm, self.c["glamat"], gm.ap, self.c["glamat"].ap)
            cmask = self.sbt("gcm", [128, 128], BF16, es)
            S.op("dve", [gm], [cmask], lambda: nc.vector.tensor_copy(cmask.ap, gm.ap[:, 2, :]))
            ones = self.sbt("gones", [128, 1], BF16, es)
            S.op("dve", [], [ones], lambda: nc.vector.memset(ones.ap, 1.0))
            wg = self.sbt("wg2", [17, 192], BF16, es, dma=True)
            S.dma("pool", wg, self.w["gla_w_gate2"], wg.ap[0:16, :], self.w["gla_w_gate2"].ap[l])
            S.dma("pool", wg, self.w["gla_b_gate"], wg.ap[16:17, :], self.w["gla_b_gate"].ap[l:l + 1, :])
            ngt = self.sbt("ngt", [128, 384], F32, es, dma=True)
            S.dma("sp", ngt, self.w["gla_norm_g"], ngt.ap, self.w["gla_norm_g"].ap[l:l + 1, :].partition_broadcast(128))
            St = self.sbt("gS", [64, 6, 64], F32, es)
            Sb = self.sbt("gSb", [64, 6, 64], BF16, es)
            S.op("dve", [], [St], lambda: nc.vector.memset(St.ap, 0.0))
            S.op("dve", [], [Sb], lambda: nc.vector.memset(Sb.ap, 0.0))
            glrT = self.sbt("glrT", [17, 128], BF16, es)
            S.op("dve", [], [glrT], lambda: nc.vector.memset(glrT.ap, 1.0))
            glr = self.sbt("glr", [128, 16], BF16, es)
            lsg = self.sbt("lsg", [128, 192], F32, es)
            lhi = self.sbt("lsghi", [128, 192], BF16, es)
            llo = self.sbt("lsglo", [128, 192], BF16, es)
            ex = [self.sbt(f"gex{i}", [128, 192], F32, es) for i in range(3)]
            qd = self.sbt("gqd", [128, 192], BF16, es)
            ki = self.sbt("gki", [128, 192], BF16, es)
            ke = self.sbt("gke", [128, 192], BF16, es)
            vt = self.sbt("gvt", [128, 384], BF16, es)
            qkT = self.sbt("gqkT", [64, 6, 128], BF16, es)
            Am = self.sbt("gAm", [128, 6, 128], BF16, es)
            dec = self.sbt("gdec", [64, 6], F32, es)
            stmp = self.sbt("gstmp", [64, 6, 64], F32, es)
            sq = self.sbt("gsq", [128, 384], F32, es)
            ss = self.sbt("gss", [128, 6], F32, es)
            sil = self.sbt("gsil", [128, 384], F32, es)
            o1 = self.sbt("go1", [128, 384], F32, es)
            go = self.sbt("gout", [128, 384], BF16, es)
            wi = self.w["w_in"]
            wv = wi.ap[l].rearrange("(k p) c -> p k c", p=128)
            gcols = ((1170, 384), (1554, 400), (1954, 384))
            Ws = [self.load_w(wi, wv[:, :, c0:c0 + cw], cw) for (c0, cw) in gcols]
            PG = Tile(self.PS[:, 0:1536], "PG", ps=True)
            PB = Tile(self.PS[:, 1536:2048], "PB", ps=True)
            PA = Tile(self.PS[:, 2048:3072], "PA", ps=True)
            PO = Tile(self.PS[:, 3072:3584], "PO", ps=True)
            PD = Tile(self.PS[:, 3584:4096], "PD", ps=True)
            S.barrier()
            for t in range(NT):
                for i, (c0_, cw) in enumerate(gcols):
                    for kc in range(KC):
                        S.op("pe", [self.hT_t[t], Ws[i]], [PG],
                             lambda kc=kc, i=i, cw=cw: nc.tensor.matmul(PG.ap[:, i * 512:i * 512 + cw], self.hT[:, kc, t * 128:(t + 1) * 128], Ws[i].ap[:, kc, 0:cw], start=(kc == 0), stop=(kc == KC - 1)),
                             inc=(kc == KC - 1 and i == 2))
                g_q, g_k, g_v, g_lr, g_og = PG.ap[:, 0:192], PG.ap[:, 192:384], PG.ap[:, 512:896], PG.ap[:, 896:912], PG.ap[:, 1024:1408]
                S.op("act", [PG], [glr], lambda: nc.scalar.copy(glr.ap, g_lr))
                S.op("act", [PG], [vt], lambda: nc.scalar.copy(vt.ap, g_v))
                S.op("act", [PG], [sil], lambda: nc.scalar.activation(out=sil.ap, in_=g_og, func=AF.Silu))
                pdb0 = PD.ap.bitcast(BF16)
                S.op("pe", [glr, self.identb], [PD], lambda: nc.tensor.transpose(pdb0[0:16, 768:896], glr.ap, self.identb.ap))
                S.op("dve", [PD], [glrT], lambda: nc.vector.tensor_copy(glrT.ap[0:16, :], pdb0[0:16, 768:896]))
                S.op("pe", [glrT, wg], [PB], lambda: nc.tensor.matmul(PB.ap[:, 0:192], glrT.ap, wg.ap, start=True, stop=True))
                S.op("act", [PB], [lsg], lambda: nc.scalar.activation(out=lsg.ap, in_=PB.ap[:, 0:192], func=AF.Exp, scale=-1.0))
                S.op("act", [lsg, self.cvec], [lsg], lambda: nc.scalar.activation(out=lsg.ap, in_=lsg.ap, func=AF.Ln, bias=self.cvec.ap[:, 2:3], scale=1.0))
                S.op("dve", [lsg], [lhi], lambda: nc.vector.tensor_copy(lhi.ap, lsg.ap))
                S.op("dve", [lsg, lhi], [llo], lambda: nc.vector.tensor_tensor(out=llo.ap, in0=lsg.ap, in1=lhi.ap, op=ALU.subtract))
                for m_ in range(2):
                    S.op("pe", [lhi, gm], [PB], lambda m_=m_: nc.tensor.matmul(PB.ap[:, m_ * 192:(m_ + 1) * 192], gm.ap[:, m_, :], lhi.ap, start=True, stop=False), inc=False)
                    S.op("pe", [llo, gm], [PB], lambda m_=m_: nc.tensor.matmul(PB.ap[:, m_ * 192:(m_ + 1) * 192], gm.ap[:, m_, :], llo.ap, start=False, stop=True), inc=False)
                for hh in range(6):
                    po_ = PB.ap[(hh % 2) * 32:(hh % 2) * 32 + 32, 384 + hh:385 + hh]
                    S.op("pe", [lhi, ones], [PB], lambda hh=hh, po_=po_: nc.tensor.matmul(po_, lhi.ap[:, hh * 32:(hh + 1) * 32], ones.ap, start=True, stop=False), inc=False)
                    S.op("pe", [llo, ones], [PB], lambda hh=hh, po_=po_: nc.tensor.matmul(po_, llo.ap[:, hh * 32:(hh + 1) * 32], ones.ap, start=False, stop=True), inc=(hh == 5))
                S.op("act", [PB], [ex[0]], lambda: nc.scalar.activation(out=ex[0].ap, in_=PB.ap[:, 0:192], func=AF.Exp, scale=-1.0 / 16))
                S.op("act", [PB], [ex[1]], lambda: nc.scalar.activation(out=ex[1].ap, in_=PB.ap[:, 0:192], func=AF.Exp, scale=1.0 / 16))
                S.op("act", [PB], [ex[2]], lambda: nc.scalar.activation(out=ex[2].ap, in_=PB.ap[:, 192:384], func=AF.Exp, scale=-1.0 / 16))
                for par in range(2):
                    S.op("act", [PB], [dec], lambda par=par: nc.scalar.activation(out=dec.ap[par * 32:par * 32 + 32, par::2], in_=PB.ap[par * 32:par * 32 + 32, 384 + par:390:2], func=AF.Exp, scale=-1.0 / 16))
                S.op("dve", [PG, ex[0]], [qd], lambda: nc.vector.scalar_tensor_tensor(out=qd.ap, in0=g_q, scalar=32 ** -0.5, in1=ex[0].ap, op0=ALU.mult, op1=ALU.mult))
                S.op("dve", [PG, ex[1]], [ki], lambda: nc.vector.tensor_tensor(out=ki.ap, in0=g_k, in1=ex[1].ap, op=ALU.mult))
                S.op("dve", [PG, ex[2]], [ke], lambda: nc.vector.tensor_tensor(out=ke.ap, in0=g_k, in1=ex[2].ap, op=ALU.mult))
                pab = PA.ap[:, 512:1024].bitcast(BF16)
                for j in range(3):
                    S.op("pe", [qd, self.identb], [PA], lambda j=j: nc.tensor.transpose(pab[0:64, j * 128:(j + 1) * 128], qd.ap[:, j * 64:(j + 1) * 64], self.identb.ap), inc=False)
                for j in range(3):
                    S.op("pe", [ki, self.identb], [PA], lambda j=j: nc.tensor.transpose(pab[0:64, (3 + j) * 128:(4 + j) * 128], ki.ap[:, j * 64:(j + 1) * 64], self.identb.ap), inc=(j == 2))
                S.op("dve", [PA], [qkT], lambda: nc.vector.tensor_copy(qkT.ap, pab[0:64, 0:768].rearrange("p (a c) -> p a c", a=6)))
                for hh in range(6):
                    r0 = (hh % 2) * 32
                    pc0 = (hh % 2) * 512 + (hh // 2) * 128
                    S.op("pe", [qkT], [PA], lambda hh=hh, r0=r0, pc0=pc0: nc.tensor.matmul(PA.ap[:, pc0:pc0 + 128], qkT.ap[r0:r0 + 32, 3 + hh // 2, :], qkT.ap[r0:r0 + 32, hh // 2, :], start=True, stop=True), inc=(hh == 5))
                for par in range(2):
                    S.op("dve", [PA, cmask], [Am], lambda par=par: nc.vector.tensor_tensor(out=Am.ap[:, par::2, :], in0=PA.ap[:, par * 512:par * 512 + 384].rearrange("p (a c) -> p a c", a=3), in1=cmask.ap.unsqueeze(1).to_broadcast([128, 3, 128]), op=ALU.mult))
                for hh in range(6):
                    r0 = (hh % 2) * 32
                    S.op("pe", [Am, vt], [PO], lambda hh=hh: nc.tensor.matmul(PO.ap[:, hh * 64:(hh + 1) * 64], Am.ap[:, hh, :], vt.ap[:, hh * 64:(hh + 1) * 64], start=True, stop=False), inc=False)
                    S.op("pe", [qkT, Sb], [PO], lambda hh=hh, r0=r0: nc.tensor.matmul(PO.ap[:, hh * 64:(hh + 1) * 64], qkT.ap[r0:r0 + 32, hh // 2, :], Sb.ap[r0:r0 + 32, hh, :], start=False, stop=True), inc=(hh == 5))
                for hh in range(6):
                    S.op("pe", [ke, vt], [PD], lambda hh=hh: nc.tensor.matmul(PD.ap[(hh % 2) * 32:(hh % 2) * 32 + 32, hh * 64:(hh + 1) * 64], ke.ap[:, hh * 32:(hh + 1) * 32], vt.ap[:, hh * 64:(hh + 1) * 64], start=True, stop=True), inc=(hh == 5))
                for par in range(2):
                    rs_ = slice(par * 32, par * 32 + 32)
                    S.op("dve", [St, dec], [stmp], lambda par=par, rs_=rs_: nc.vector.tensor_tensor(out=stmp.ap[rs_, par::2, :], in0=St.ap[rs_, par::2, :], in1=dec.ap[rs_, par::2].unsqueeze(2).to_broadcast([32, 3, 64]), op=ALU.mult))
                    S.op("dve", [stmp, PD], [St], lambda par=par, rs_=rs_: nc.vector.tensor_tensor(out=St.ap[rs_, par::2, :], in0=stmp.ap[rs_, par::2, :], in1=PD.ap[rs_, 0:384].rearrange("p (a c) -> p a c", a=6)[:, par::2, :], op=ALU.add))
                S.op("act", [PO], [sq], lambda: nc.scalar.activation(out=sq.ap, in_=PO.ap[:, 0:384], func=AF.Square))
                S.op("dve", [sq], [ss], lambda: nc.vector.tensor_reduce(out=ss.ap, in_=sq.ap.rearrange("p (a c) -> p a c", a=6), axis=AX.X, op=ALU.add))
                S.op("act", [ss, self.cvec], [ss], lambda: nc.scalar.activation(out=ss.ap, in_=ss.ap, func=AF.Sqrt, bias=self.cvec.ap[:, 1:2], scale=1.0 / 64))
                S.op("dve", [ss], [ss], lambda: nc.vector.reciprocal(ss.ap, ss.ap))
                S.op("dve", [PO, ss], [o1], lambda: nc.vector.tensor_tensor(out=o1.ap.rearrange("p (a c) -> p a c", a=6), in0=PO.ap[:, 0:384].rearrange("p (a c) -> p a c", a=6), in1=ss.ap.unsqueeze(2).to_broadcast([128, 6, 64]), op=ALU.mult))
                S.op("dve", [o1, ngt], [o1], lambda: nc.vector.tensor_tensor(out=o1.ap, in0=o1.ap, in1=ngt.ap, op=ALU.mult))
                S.op("dve", [o1, sil], [go], lambda: nc.vector.tensor_tensor(out=go.ap, in0=o1.ap, in1=sil.ap, op=ALU.mult))
                for par in range(2):
                    S.op("act", [St], [Sb], lambda par=par: nc.scalar.copy(Sb.ap[par * 32:par * 32 + 32, par::2, :], St.ap[par * 32:par * 32 + 32, par::2, :]))
                pdb = PD.ap.bitcast(BF16)
                for j in range(3):
                    S.op("pe", [go, self.identb], [PD], lambda j=j: nc.tensor.transpose(pdb[:, 384 + j * 128:384 + (j + 1) * 128], go.ap[:, j * 128:(j + 1) * 128], self.identb.ap), inc=(j == 2))
                S.op("act", [PD], [self.mT_t[t]], lambda: nc.scalar.copy(self.mT[:, 3:6, t * 128:(t + 1) * 128], pdb[:, 384:768].rearrange("p (a c) -> p a c", a=3)))
            S.barrier()

    def nsa_phase(self, l):
        S, nc = self.S, self.nc
        with ExitStack() as es:
            qT = self.sb("qT", [128, 3, SEQ], BF16, es)
            qT_g = [Tile(qT[:, :, qg * 512:(qg + 1) * 512], f"qT{qg}") for qg in range(4)]
            kT = self.sb("kT12", [128, 2, SEQ], BF16, es)
            kT_b = [None] + [Tile(kT[:, br, :], f"kT{br + 1}") for br in range(2)]
            Va = self.sb("Vaug", [128, NT * 4, 65], BF16, es)
            Va_t = [Tile(Va[:, t * 4:(t + 1) * 4, :], f"Va{t}") for t in range(NT)]
            Va_all = Tile(Va, "Va_all")
            gates = self.sbt("gates", [128, NT, 18], F32, es)
            S.op("dve", [], [Va_all], lambda: nc.vector.memset(Va[:, :, 64:65], 1.0))
            for t in range(NT):
                Va_t[t].lw = Va_all.lw
            kcT2 = [self.sbt(f"kcT2_{g}", [128, 128], BF16, es) for g in range(2)]
            vca = [self.sbt(f"vca_{g}", [128, 97], BF16, es, dma=True) for g in range(2)]
            selTt = self.sb("selT", [64, SEQ], BF16, es)
            selT = [Tile(selTt[g * 32:(g + 1) * 32, :], f"selT{g}") for g in range(2)]
            with ExitStack() as es1:
                self.nsa_inproj(l, es1, qT, qT_g, kT, kT_b, Va, Va_t, gates, kcT2, vca)
            S.barrier()
            with ExitStack() as es2:
                self.nsa_attend(l, es2, qT, qT_g, kT, kT_b, Va, Va_t, gates, kcT2, vca, selT)
            S.barrier()

    def nsa_inproj(self, l, es, qT, qT_g, kT, kT_b, Va, Va_t, gates, kcT2, vca):
        S, nc = self.S, self.nc
        v0T = self.sbt("v0T", [128, SEQ], BF16, es)
        k0T = self.sbt("k0T", [128, SEQ], BF16, es)
        kT_b = [k0T] + kT_b[1:]
        es_outer = es
        es = es_outer.enter_context(ExitStack())
        rope = self.sbt("rope", [128, 4, NT, 8], F32, es, dma=True)
        S.dma("sp", rope, self.c["rope"], rope.ap, self.c["rope"].ap)
        qtok = [self.sbt(f"qtok{i}", [128, 384], BF16, es) for i in range(2)]
        ktok = [self.sbt(f"ktok{i}", [128, 3, 128], BF16, es) for i in range(2)]
        rt = [self.sbt(f"ropet{i}", [128, 4, 6, 8], F32, es) for i in range(2)]
        wi_ = self.w["w_in"]
        wv = wi_.ap[l].rearrange("(k p) c -> p k c", p=128)

        def bc(tab, t, shp):
            a = rope.ap[:, tab, t, :]
            for _ in range(len(shp) - 2):
                a = a.unsqueeze(1)
            return a.to_broadcast(shp)

        def do_rope(t, x, out, tabc, tabs, shp, tmp, reads, outt):
            pre = (slice(None),) * (len(shp) - 1)
            x1, x2 = x[pre + (slice(0, 8),)], x[pre + (slice(8, 16),)]
            out1, out2 = out[pre + (slice(0, 8),)], out[pre + (slice(8, 16),)]
            n = shp[1] * (shp[2] if len(shp) == 4 else 1)
            tv = [tmp.ap[:, i, 0:n, :] for i in range(4)]
            if len(shp) == 4:
                tv = [a.rearrange("p (a b) c -> p a b c", a=shp[1]) for a in tv]
            S.op("dve", reads + [rope, outt], [tmp], lambda: nc.vector.tensor_tensor(out=tv[0], in0=x1, in1=bc(tabc, t, shp), op=ALU.mult))
            S.op("dve", reads + [rope], [tmp], lambda: nc.vector.tensor_tensor(out=tv[1], in0=x2, in1=bc(tabs, t, shp), op=ALU.mult))
            S.op("dve", reads + [rope], [tmp], lambda: nc.vector.tensor_tensor(out=tv[2], in0=x1, in1=bc(tabs, t, shp), op=ALU.mult))
            S.op("dve", reads + [rope], [tmp], lambda: nc.vector.tensor_tensor(out=tv[3], in0=x2, in1=bc(tabc, t, shp), op=ALU.mult))
            S.op("dve", [tmp], [outt], lambda: nc.vector.tensor_tensor(out=out1, in0=tv[0], in1=tv[1], op=ALU.subtract))
            S.op("dve", [tmp], [outt], lambda: nc.vector.tensor_tensor(out=out2, in0=tv[2], in1=tv[3], op=ALU.add))

        for wi, (c0, cw) in enumerate(((0, 512), (512, 512), (1024, 146))):
            Wt = self.load_w(wi_, wv[:, :, c0:c0 + cw], cw)
            pend = None
            for t in range(NT):
                bk = self.next_bank()
                for kc in range(KC):
                    S.op("pe", [self.hT_t[t], Wt], [bk],
                         lambda kc=kc, bk=bk, t=t: nc.tensor.matmul(bk.ap[:, 0:cw], self.hT[:, kc, t * 128:(t + 1) * 128], Wt.ap[:, kc, 0:cw], start=(kc == 0), stop=(kc == KC - 1)),
                         inc=(kc == KC - 1))
                tsl = slice(t * 128, (t + 1) * 128)
                if wi == 0:
                    qt_, kt_, tmp = qtok[t % 2], ktok[t % 2], rt[t % 2]
                    qsrc = bk.ap[:, 0:384].rearrange("p (a d) -> p a d", a=6)
                    qdst = qt_.ap.rearrange("p (a d) -> p a d", a=6)
                    S.op("act", [bk], [qt_], lambda bk=bk, qt_=qt_: nc.scalar.activation(out=qt_.ap, in_=bk.ap[:, 0:384], func=AF.Copy, scale=0.125))
                    do_rope(t, qsrc, qdst, 0, 1, [128, 6, 8], tmp, [bk], qt_)
                    ksrc = bk.ap[:, 384:512].rearrange("p (g d) -> p g d", g=2)
                    kdst = kt_.ap[:, 0, :].rearrange("p (g d) -> p g d", g=2)
                    S.op("act", [bk], [kt_], lambda ksrc=ksrc, kdst=kdst: nc.scalar.copy(kdst, ksrc))
                    do_rope(t, ksrc, kdst, 2, 3, [128, 2, 8], tmp, [bk], kt_)

                    def tr(t=t, qt_=qt_, kt_=kt_, tsl=tsl):
                        tb_ = self.next_bank()
                        pb = tb_.ap.bitcast(BF16)
                        for hh in range(6):
                            g_, h_ = hh // 3, hh % 3
                            S.op("pe", [qt_, self.identb], [tb_], lambda hh=hh, g_=g_, h_=h_: nc.tensor.transpose(pb[g_ * 64:(g_ + 1) * 64, h_ * 128:(h_ + 1) * 128], qt_.ap[:, hh * 64:(hh + 1) * 64], self.identb.ap), inc=False)
                        S.op("pe", [kt_, self.identb], [tb_], lambda: nc.tensor.transpose(pb[:, 384:512], kt_.ap[:, 0, :], self.identb.ap))
                        S.op("act", [tb_], [qT_g[t // 4]], lambda: nc.scalar.copy(qT[:, :, tsl], pb[:, 0:384].rearrange("p (a c) -> p a c", a=3)))
                        S.op("act", [tb_], [kT_b[0]], lambda: nc.scalar.copy(k0T.ap[:, tsl], pb[:, 384:512]))
                elif wi == 1:
                    kt_, tmp = ktok[t % 2], rt[t % 2]
                    S.op("act", [bk], [kt_], lambda bk=bk, kt_=kt_: nc.scalar.copy(kt_.ap[:, 0, :], bk.ap[:, 0:128]))
                    for b_ in range(2):
                        ksrc = bk.ap[:, 128 + 256 * b_:256 + 256 * b_].rearrange("p (g d) -> p g d", g=2)
                        kdst = kt_.ap[:, 1 + b_, :].rearrange("p (g d) -> p g d", g=2)
                        S.op("act", [bk], [kt_], lambda ksrc=ksrc, kdst=kdst: nc.scalar.copy(kdst, ksrc))
                        do_rope(t, ksrc, kdst, 2, 3, [128, 2, 8], tmp, [bk], kt_)
                    S.op("act", [bk], [Va_t[t]], lambda bk=bk, t=t: nc.scalar.copy(Va[:, t * 4:t * 4 + 2, 0:64], bk.ap[:, 256:384].rearrange("p (g d) -> p g d", g=2)))

                    def tr(t=t, kt_=kt_, tsl=tsl):
                        tb_ = self.next_bank()
                        pb = tb_.ap.bitcast(BF16)
                        for j in range(3):
                            S.op("pe", [kt_, self.identb], [tb_], lambda j=j: nc.tensor.transpose(pb[:, j * 128:(j + 1) * 128], kt_.ap[:, j, :], self.identb.ap), inc=(j == 2))
                        S.op("act", [tb_], [v0T], lambda: nc.scalar.copy(v0T.ap[:, tsl], pb[:, 0:128]))
                        S.op("act", [tb_], [kT_b[1]], lambda: nc.scalar.copy(kT[:, 0, tsl], pb[:, 128:256]))
                        S.op("act", [tb_], [kT_b[2]], lambda: nc.scalar.copy(kT[:, 1, tsl], pb[:, 256:384]))
                else:
                    S.op("act", [bk], [Va_t[t]], lambda bk=bk, t=t: nc.scalar.copy(Va[:, t * 4 + 2:t * 4 + 4, 0:64], bk.ap[:, 0:128].rearrange("p (g d) -> p g d", g=2)))
                    S.op("act", [bk], [gates], lambda bk=bk, t=t: nc.scalar.activation(out=gates.ap[:, t, :], in_=bk.ap[:, 128:146], func=AF.Sigmoid))
                    tr = None
                if pend is not None:
                    pend()
                pend = tr
            if pend is not None:
                pend()
        S.barrier()
        es.close()
        es = es_outer
        self.mark("compress")
        w1sb = self.sbt("w1sb", [128, 2, 32, 64], BF16, es, dma=True)
        cw1 = self.w["cmp_w1"]
        for kv in range(2):
            for hf in range(2):
                S.dma("pool", w1sb, cw1, w1sb.ap[hf * 64:(hf + 1) * 64, kv], cw1.ap[l, kv].rearrange("(j d) h -> d j h", d=64))
        possb = self.sbt("possb", [32, 2, 64], BF16, es, dma=True)
        S.dma("pool", possb, self.w["cmp_pos"], possb.ap, self.w["cmp_pos"].ap[l].rearrange("k j d -> j k d"))
        w2sb = self.sbt("w2sb", [64, 3, 64], BF16, es, dma=True)
        cw2 = self.w["cmp_w2"]
        S.dma("pool", w2sb, cw2, w2sb.ap[:, 0, :], cw2.ap[l, 0])
        S.dma("pool", w2sb, cw2, w2sb.ap[:, 1, :], cw2.ap[l, 0])
        S.dma("pool", w2sb, cw2, w2sb.ap[:, 2, :], cw2.ap[l, 1])
        for g in range(2):
            S.dma("pool", vca[g], self.c["c2s"], vca[g].ap[:, 65:97], self.c["c2s"].ap)
            S.op("dve", [], [vca[g]], lambda g=g: nc.vector.memset(vca[g].ap[:, 64:65], 1.0))
        posT = self.sbt("posT", [64, 2, 32], BF16, es)
        bk = self.next_bank()
        for kv in range(2):
            S.op("pe", [possb, self.identb], [bk], lambda kv=kv: nc.tensor.transpose(bk.ap.bitcast(BF16)[0:64, kv * 32:(kv + 1) * 32], possb.ap[:, kv, :], self.identb.ap[0:32, 0:32]), inc=(kv == 1))
        S.op("dve", [bk], [posT], lambda: nc.vector.tensor_copy(posT.ap, bk.ap.bitcast(BF16)[0:64, 0:64].rearrange("p (a c) -> p a c", a=2)))
        posb = self.sbt("posb", [64, 2], F32, es)
        bk2 = self.next_bank()
        for kv in range(2):
            for j in range(32):
                S.op("pe", [w1sb, posT], [bk2], lambda kv=kv, j=j: nc.tensor.matmul(bk2.ap[0:64, kv:kv + 1], w1sb.ap[0:64, kv, j, :], posT.ap[:, kv, j:j + 1], start=(j == 0), stop=(j == 31)), inc=(kv == 1 and j == 31))
        S.op("dve", [bk2], [posb], lambda: nc.vector.tensor_copy(posb.ap, bk2.ap[0:64, 0:2]))
        hid = [self.sbt(f"chid{i}", [64, 128], BF16, es) for i in range(2)]
        hi = 0
        for g in range(2):
            gs = slice(g * 64, (g + 1) * 64)
            for kv in range(2):
                src_t = kT_b[0] if kv == 0 else v0T
                src = k0T.ap if kv == 0 else v0T.ap
                hp = self.next_bank()
                for j in range(32):
                    S.op("pe", [w1sb, src_t], [hp], lambda j=j, hp=hp, kv=kv, src=src: nc.tensor.matmul(hp.ap[0:64, 0:NCMP], w1sb.ap[gs, kv, j, :], src[gs, j:j + 16 * (NCMP - 1) + 1:16], start=(j == 0), stop=(j == 31)), inc=(j == 31))
                hd = hid[hi % 2]
                hi += 1
                S.op("act", [hp, posb], [hd], lambda hp=hp, hd=hd, kv=kv: nc.scalar.activation(out=hd.ap[:, 0:NCMP], in_=hp.ap[0:64, 0:NCMP], func=AF.Gelu_apprx_tanh, bias=posb.ap[:, kv:kv + 1], scale=1.0))
                op_ = self.next_bank()
                if kv == 0:
                    S.op("pe", [hd, w2sb], [op_], lambda hd=hd, op_=op_: nc.tensor.matmul(op_.ap[:, 0:NCMP], w2sb.ap[:, 0:2, :].rearrange("p a c -> p (a c)"), hd.ap[:, 0:NCMP], start=True, stop=True))
                    S.op("dve", [op_], [kcT2[g]], lambda op_=op_, g=g: nc.vector.tensor_copy(kcT2[g].ap[:, 0:NCMP], op_.ap[:, 0:NCMP]))
                else:
                    S.op("pe", [hd, w2sb], [op_], lambda hd=hd, op_=op_: nc.tensor.matmul(op_.ap[0:NCMP, 0:64], hd.ap[:, 0:NCMP], w2sb.ap[:, 2, :], start=True, stop=True))
                    S.op("dve", [op_], [vca[g]], lambda op_=op_, g=g: nc.vector.tensor_copy(vca[g].ap[0:NCMP, 0:64], op_.ap[0:NCMP, 0:64]))

    def nsa_attend(self, l, es, qT, qT_g, kT, kT_b, Va, Va_t, gates, kcT2, vca, selT):
        S, nc = self.S, self.nc
        cmpm = self.sbt("cmpm", [128, SEQ], BF16, es, dma=True)
        S.dma("pool", cmpm, self.c["cmpmask"], cmpm.ap, self.c["cmpmask"].ap)
        pbuf = [self.sbt(f"pbuf{i}", [128, 512], BF16, es) for i in range(3)]
        es_outer = es
        es = es_outer.enter_context(ExitStack())
        frc = self.sbt("force", [128, 2, NT, 32], BF16, es, dma=True)
        S.dma("pool", frc, self.c["force"], frc.ap, self.c["force"].ap)
        prr = [0]
        sbanks = self.bank[0:2]
        srr = [0]
        obanks = self.bank[2:8]

        def next_s():
            b = sbanks[srr[0] % 2]
            srr[0] += 1
            return b

        def next_p():
            p = pbuf[prr[0] % 3]
            prr[0] += 1
            return p

        self.mark("pass1")
        impacc = self.sbt("impacc", [128, NT, 32], F32, es)
        impf = impacc
        rep = self.sbt("rep", [128, NT, 32], F32, es)
        rep1 = self.sbt("rep1", [128, 32], F32, es)
        m8 = self.sbt("m8", [128, 8], F32, es)
        selb = self.sbt("selb", [128, NT, 32], BF16, es)
        rden = self.sbt("rden", [128, 4], F32, es)
        itmp = self.sbt("itmp", [128, 4, 32], F32, es)
        orr = 0
        for g in range(2):
            gs = slice(g * 64, (g + 1) * 64)
            for h in range(3):
                for qg in range(4):
                    qs = slice(qg * 512, (qg + 1) * 512)
                    sbk = next_s()
                    S.op("pe", [kcT2[g], qT_g[qg]], [sbk], lambda sbk=sbk, h=h, qs=qs: nc.tensor.matmul(sbk.ap[0:NCMP, :], kcT2[g].ap[gs, 0:NCMP], qT[gs, h, qs], start=True, stop=True))
                    pc = next_p()
                    S.op("act", [sbk], [pc], lambda sbk=sbk, pc=pc: nc.scalar.activation(out=pc.ap[0:NCMP, :], in_=sbk.ap[0:NCMP, :], func=AF.Exp))
                    S.op("dve", [pc, cmpm], [pc], lambda pc=pc, qs=qs: nc.vector.tensor_tensor(out=pc.ap[0:NCMP, :], in0=pc.ap[0:NCMP, :], in1=cmpm.ap[0:NCMP, qs], op=ALU.mult))
                    ib = obanks[orr % 6]
                    orr += 1
                    for qt in range(4):
                        S.op("pe", [pc, vca[g]], [ib], lambda qt=qt, ib=ib, pc=pc: nc.tensor.matmul(ib.ap[:, qt * 33:(qt + 1) * 33], pc.ap[0:NCMP, qt * 128:(qt + 1) * 128], vca[g].ap[0:NCMP, 64:97], start=True, stop=True), inc=(qt == 3))
                    ibv = ib.ap[:, 0:132].rearrange("p (a c) -> p a c", a=4)
                    S.op("dve", [ib], [rden], lambda ibv=ibv: nc.vector.tensor_scalar_max(rden.ap, ibv[:, :, 0], 1e-30))
                    S.op("dve", [rden], [rden], lambda: nc.vector.reciprocal(rden.ap, rden.ap))
                    acc = impacc.ap[:, 4 * qg:4 * qg + 4, :]
                    rb = rden.ap.unsqueeze(2).to_broadcast([128, 4, 32])
                    if h == 0:
                        S.op("dve", [ib, rden], [impacc], lambda ibv=ibv, acc=acc, rb=rb: nc.vector.tensor_tensor(out=acc, in0=ibv[:, :, 1:33], in1=rb, op=ALU.mult))
                    else:
                        S.op("dve", [ib, rden], [itmp], lambda ibv=ibv, rb=rb: nc.vector.tensor_tensor(out=itmp.ap, in0=ibv[:, :, 1:33], in1=rb, op=ALU.mult))
                        S.op("dve", [itmp, impacc], [impacc], lambda acc=acc: nc.vector.tensor_tensor(out=acc, in0=acc, in1=itmp.ap, op=ALU.add))
            self.mark(f"topk{g}")
            S.op("dve", [impacc, frc], [impf], lambda: nc.vector.tensor_tensor(out=impf.ap, in0=impacc.ap, in1=frc.ap[:, 0], op=ALU.mult))
            S.op("dve", [impf, frc], [impf], lambda: nc.vector.tensor_tensor(out=impf.ap, in0=impf.ap, in1=frc.ap[:, 1], op=ALU.add))
            for t in range(NT):
                S.op("dve", [impf], [m8], lambda t=t: nc.vector.max(out=m8.ap, in_=impf.ap[:, t, :]))
                S.op("dve", [impf, m8], [rep1], lambda t=t: nc.vector.match_replace(out=rep1.ap, in_to_replace=m8.ap, in_values=impf.ap[:, t, :], imm_value=-1e9))
                S.op("dve", [rep1], [m8], lambda: nc.vector.max(out=m8.ap, in_=rep1.ap))
                S.op("dve", [rep1, m8], [rep], lambda t=t: nc.vector.match_replace(out=rep.ap[:, t, :], in_to_replace=m8.ap, in_values=rep1.ap, imm_value=-1e9))
            S.op("dve", [impf, rep], [rep], lambda: nc.vector.tensor_tensor(out=rep.ap, in0=impf.ap, in1=rep.ap, op=ALU.subtract))
            S.op("dve", [rep], [rep], lambda: nc.vector.tensor_scalar(rep.ap, rep.ap, 1.0, -NEG, op0=ALU.min, op1=ALU.mult))
            S.op("dve", [rep], [selb], lambda: nc.vector.tensor_scalar_add(selb.ap, rep.ap, NEG))
            for half in range(2):
                tb_ = next_s()
                pb = tb_.ap.bitcast(BF16)
                for tt in range(8):
                    t = half * 8 + tt
                    S.op("pe", [selb, self.identb], [tb_], lambda t=t, tt=tt, pb=pb, g=g: nc.tensor.transpose(pb[g * 32:(g + 1) * 32, tt * 128:(tt + 1) * 128], selb.ap[:, t, :], self.identb.ap), inc=(tt == 7))
                S.op("act", [tb_], [selT[g]], lambda half=half, pb=pb, g=g: nc.scalar.copy(selT[g].ap[:, half * 1024:(half + 1) * 1024], pb[g * 32:(g + 1) * 32, :]))
        S.barrier()
        es.close()
        es = es_outer
        self.mark("pass2")
        tri = self.sbt("tri", [128, 2, 128], BF16, es, dma=True)
        S.dma("pool", tri, self.c["tri"], tri.ap, self.c["tri"].ap)
        efull = self.sbt("efull", [64, SEQ], BF16, es, dma=True)
        for g in range(2):
            S.dma("pool", efull, self.c["efull"], efull.ap[g * 32:(g + 1) * 32, :], self.c["efull"].ap)
        acc = self.sbt("oacc", [128, 4, 64], F32, es)
        otmp = self.sbt("otmp", [128, 4, 64], F32, es)
        ocomb = [self.sbt(f"ocomb{i}", [128, 4, 64], BF16, es) for i in range(2)]
        den = self.sbt("oden", [128, 3, 4], F32, es)
        oset = 0
        for g in range(2):
            gs = slice(g * 64, (g + 1) * 64)
            for h in range(3):
                hh = g * 3 + h
                chunk, half = hh // 2, hh % 2
                for qg in range(4):
                    qs = slice(qg * 512, (qg + 1) * 512)
                    Ob = obanks[(oset % 2) * 3:(oset % 2) * 3 + 3]
                    oset += 1
                    ofresh = [True, True, True]
                    jobs = [("cmp", 0)] + [("slc", kt) for kt in range(0, 4 * qg + 4)] + [("win", kt) for kt in range(max(0, 4 * qg - 4), 4 * qg + 4)]
                    pend = None
                    for kind, kt in jobs:
                        sbk = next_s()
                        pt = next_p()
                        ks = slice(kt * 128, (kt + 1) * 128)
                        if kind == "cmp":
                            S.op("pe", [kcT2[g], qT_g[qg]], [sbk], lambda sbk=sbk: nc.tensor.matmul(sbk.ap[0:NCMP, :], kcT2[g].ap[gs, 0:NCMP], qT[gs, h, qs], start=True, stop=True))
                            S.op("act", [sbk], [pt], lambda sbk=sbk, pt=pt: nc.scalar.activation(out=pt.ap[0:NCMP, :], in_=sbk.ap[0:NCMP, :], func=AF.Exp))
                            S.op("dve", [pt, cmpm], [pt], lambda pt=pt: nc.vector.tensor_tensor(out=pt.ap[0:NCMP, :], in0=pt.ap[0:NCMP, :], in1=cmpm.ap[0:NCMP, qs], op=ALU.mult))

                            def pv(pt=pt):
                                for qt in range(4):
                                    S.op("pe", [pt, vca[g]], [Ob[0]], lambda qt=qt: nc.tensor.matmul(Ob[0].ap[:, qt * 65:(qt + 1) * 65], pt.ap[0:NCMP, qt * 128:(qt + 1) * 128], vca[g].ap[0:NCMP, 0:65], start=(qt == 0), stop=True), inc=(qt == 3))
                        else:
                            br = 1 if kind == "slc" else 2
                            qlo = max(4 * qg, kt)
                            qhi = 4 * qg + 4 if kind == "slc" else min(kt + 4, 4 * qg + 3) + 1
                            ncols = (qhi - qlo) * 128
                            qsl = slice(qlo * 128, qhi * 128)
                            mms = [(kT[gs, br - 1, ks], qT[gs, h, qsl], 0, ncols, [kT_b[br], qT_g[qg]])]
                            if kind == "slc" and qg >= 2:
                                mms.append((efull.ap[g * 32:(g + 1) * 32, ks], selT[g].ap[:, qsl], 0, ncols, [efull, selT[g]]))
                            if kt >= 4 * qg:
                                mms.append((self.identb.ap, tri.ap[:, 0, :], 0, 128, [self.identb, tri]))
                            if kind == "win" and 4 * qg <= kt + 4 <= 4 * qg + 3:
                                c0 = (kt + 4 - qlo) * 128
                                mms.append((self.identb.ap, tri.ap[:, 1, :], c0, 128, [self.identb, tri]))
                            for i, (lt, rh, c0, nn_, rds) in enumerate(mms):
                                S.op("pe", rds, [sbk], lambda lt=lt, rh=rh, c0=c0, nn_=nn_, i=i, sbk=sbk, n_=len(mms): nc.tensor.matmul(sbk.ap[:, c0:c0 + nn_], lt, rh, start=(i == 0), stop=(i == n_ - 1)), inc=(i == len(mms) - 1))
                            S.op("act", [sbk], [pt], lambda sbk=sbk, pt=pt, ncols=ncols: nc.scalar.activation(out=pt.ap[:, 0:ncols], in_=sbk.ap[:, 0:ncols], func=AF.Exp))
                            ob = Ob[br]
                            vb = br - 1

                            def pv(pt=pt, kind=kind, kt=kt, qlo=qlo, qhi=qhi, ob=ob, vb=vb, br=br, ofresh=ofresh):
                                for qt in range(qlo, qhi):
                                    first = ofresh[br]
                                    ofresh[br] = False
                                    ql = qt - 4 * qg
                                    S.op("pe", [pt, Va_t[kt]], [ob], lambda qt=qt, ql=ql, first=first: nc.tensor.matmul(ob.ap[:, ql * 65:(ql + 1) * 65], pt.ap[:, (qt - qlo) * 128:(qt - qlo + 1) * 128], Va[:, kt * 4 + vb * 2 + g, :], start=first, stop=(kt == qt)), inc=(qt == qhi - 1))
                        if pend is not None:
                            pend()
                        pend = pv
                    pend()
                    oc = ocomb[oset % 2]
                    for b in range(3):
                        obv = Ob[b].ap[:, 0:260].rearrange("p (a c) -> p a c", a=4)
                        S.op("dve", [Ob[b]], [den], lambda obv=obv, b=b: nc.vector.tensor_scalar_max(den.ap[:, b, :], obv[:, :, 64], 1e-30))
                        S.op("dve", [den], [den], lambda b=b: nc.vector.reciprocal(den.ap[:, b, :], den.ap[:, b, :]))
                        S.op("dve", [den, gates], [den], lambda b=b: nc.vector.tensor_tensor(out=den.ap[:, b, :], in0=den.ap[:, b, :], in1=gates.ap[:, 4 * qg:4 * qg + 4, g * 9 + h * 3 + b], op=ALU.mult))
                        cb = den.ap[:, b, :].unsqueeze(2).to_broadcast([128, 4, 64])
                        if b == 0:
                            S.op("dve", [Ob[b], den], [acc], lambda obv=obv, cb=cb: nc.vector.tensor_tensor(out=acc.ap, in0=obv[:, :, 0:64], in1=cb, op=ALU.mult))
                        else:
                            S.op("dve", [Ob[b], den], [otmp], lambda obv=obv, cb=cb: nc.vector.tensor_tensor(out=otmp.ap, in0=obv[:, :, 0:64], in1=cb, op=ALU.mult))
                            dst = acc if b == 1 else oc
                            S.op("dve", [otmp, acc], [dst], lambda dst=dst: nc.vector.tensor_tensor(out=dst.ap, in0=acc.ap, in1=otmp.ap, op=ALU.add))
                    tb_ = next_s()
                    pb = tb_.ap.bitcast(BF16)
                    hs = slice(half * 64, (half + 1) * 64)
                    for qt in range(4):
                        S.op("pe", [oc, self.identb], [tb_], lambda qt=qt, pb=pb, oc=oc: nc.tensor.transpose(pb[hs, qt * 128:(qt + 1) * 128], oc.ap[:, qt, :], self.identb.ap), inc=(qt == 3))
                    S.op("act", [tb_], [self.mT_t[4 * qg + i] for i in range(4)], lambda pb=pb, chunk=chunk: nc.scalar.copy(self.mT[hs, chunk, qs], pb[hs, 0:512]))


_CACHE = {}


def _host_inputs(inputs, n_cores, n_seq):
    consts = _consts()
    maps = []
    x = np.ascontiguousarray(inputs["x"], dtype=np.float32)
    for c in range(n_cores):
        m = {"x": np.ascontiguousarray(x[c * n_seq:(c + 1) * n_seq])}
        for k in WEIGHT_SHAPES:
            m[k] = np.ascontiguousarray(inputs[k], dtype=np.float32)
        for k, v in consts.items():
            m["c_" + k] = np.ascontiguousarray(v, dtype=np.float32)
        maps.append(m)
    return maps


def kernel(**inputs):
    n_cores, n_seq = 8, 2
    b = Builder(n_seq=n_seq)
    nc = b.build()
    maps = _host_inputs(inputs, n_cores, n_seq)
    res = run_bass_kernel_spmd(nc, maps, core_ids=list(range(n_cores)))
    out = np.concatenate([np.asarray(r["out"]) for r in res.results], axis=0)
    return out.astype(np.float32)
```

```python
import math
import numpy as np
from contextlib import ExitStack
import concourse.bass as bass
import concourse.mybir as mybir
from concourse.bass_utils import run_bass_kernel_spmd
from concourse.ap import AP

F32 = mybir.dt.float32
BF16 = mybir.dt.bfloat16
AF = mybir.ActivationFunctionType
ALU = mybir.AluOpType
AX = mybir.AxisListType

SEQ = 2048
NT = 16
D = 1024
KC = 8
DEPTH = 4
P_IN = 2594
D_FF = 4096
ALPHA = (2 * DEPTH) ** 0.25
LN_EPS = 1e-5
RMS_EPS = 1e-6
NEG = -30000.0
NCMP = 127


class Tile:
    __slots__ = ("ap", "lw", "rd", "dsem", "dcnt", "name", "ps")

    def __init__(self, ap, name="", ps=False):
        self.ap = ap
        self.ps = ps
        self.lw = None
        self.rd = {}
        self.dsem = None
        self.dcnt = 0
        self.name = name

    def __getitem__(self, k):
        return self.ap[k]


class Sched:
    def __init__(self, nc, es):
        self.nc = nc
        self.es = es
        self.eng = {"pe": nc.tensor, "act": nc.scalar, "dve": nc.vector, "pool": nc.gpsimd, "sp": nc.sync}
        self.sem = {}
        self.cnt = {}
        for e in ("pe", "act", "dve", "pool"):
            self.sem[e] = es.enter_context(nc.semaphore("sem_" + e))
            self.cnt[e] = 0
        self.waited = {e: {} for e in self.eng}
        self.nops = 0
        self.nwaits = 0

    def attach_dma_sem(self, tile, name=None):
        name = name or ("d_" + tile.name)
        if name not in self.sem:
            self.sem[name] = self.es.enter_context(self.nc.semaphore(name))
        tile.dsem = name
        return tile

    def share_dma_sem(self, tile, other):
        tile.dsem = other.dsem
        return tile

    def _sync(self, eng, reads, writes):
        needs = {}

        def need(k, v):
            if eng == "pe" and k == "pe":
                return
            if needs.get(k, 0) < v:
                needs[k] = v

        for t in reads:
            if t.lw is not None:
                need(*t.lw)
            if t.ps:
                for k, v in t.rd.items():
                    if k != eng:
                        need(k, v)
        for t in writes:
            if t.lw is not None and t.lw[0] != eng:
                need(*t.lw)
            for k, v in t.rd.items():
                if k != eng:
                    need(k, v)
        w = self.waited[eng]
        for k, v in needs.items():
            if w.get(k, 0) < v:
                self.eng[eng].wait_ge(self.sem[k], v)
                w[k] = v
                self.nwaits += 1

    max_ops = None

    def op(self, eng, reads, writes, fn, inc=True):
        if self.max_ops is not None and self.nops >= self.max_ops:
            return None
        self._sync(eng, reads, writes)
        ins = fn()
        self.nops += 1
        if inc:
            self.cnt[eng] += 1
            ins.then_inc(self.sem[eng], 1)
            val = self.cnt[eng]
        else:
            val = self.cnt[eng] + 1
        for t in reads:
            if t.rd.get(eng, 0) < val:
                t.rd[eng] = val
        for t in writes:
            t.lw = (eng, val)
            t.rd = {}
        return ins

    def dma(self, q, out_t, in_t, out_ap, in_ap, **kw):
        self._sync(q, [in_t], [out_t])
        ins = self.eng[q].dma_start(out=out_ap, in_=in_ap, **kw)
        key = out_t.dsem
        base = self._dcnt.get(key, 0) + 16
        self._dcnt[key] = base
        ins.then_inc(self.sem[key], 16)
        self.nops += 1
        in_t.rd[key] = base
        out_t.lw = (key, base)
        out_t.rd = {}
        return ins

    _dcnt = None

    def wait_tile(self, eng, t):
        self._sync(eng, [t], [])

    def barrier(self):
        for e in ("pe", "act", "dve", "pool", "sp"):
            w = self.waited[e]
            for f in ("pe", "act", "dve", "pool"):
                if f != e and w.get(f, 0) < self.cnt[f]:
                    self.eng[e].wait_ge(self.sem[f], self.cnt[f])
                    w[f] = self.cnt[f]


def run_pipeline(gens, start_every=1):
    active = []
    it = iter(gens)
    more = True
    rnd = 0
    while more or active:
        rnd += 1
        if more and (rnd - 1) % start_every == 0:
            try:
                active.append(next(it))
            except StopIteration:
                more = False
        nxt = []
        for g in active:
            try:
                next(g)
                nxt.append(g)
            except StopIteration:
                pass
        active = nxt


def _consts():
    c = {}
    c["ident"] = np.eye(128, dtype=np.float32)
    pos = np.arange(SEQ, dtype=np.float32)
    half = 8
    inv = (np.float32(500000.0) ** (-np.arange(half, dtype=np.float32) * np.float32(2.0) / np.float32(16))).astype(np.float32)
    ang = (pos[:, None] * inv[None, :]).astype(np.float32)
    cos = np.cos(ang).astype(np.float32)
    sin = np.sin(ang).astype(np.float32)
    rope = np.zeros((128, 4, NT, 8), np.float32)
    cs = cos.reshape(NT, 128, 8).transpose(1, 0, 2)
    sn = sin.reshape(NT, 128, 8).transpose(1, 0, 2)
    rope[:, 0] = cs * 0.125
    rope[:, 1] = sn * 0.125
    rope[:, 2] = cs
    rope[:, 3] = sn
    c["rope"] = rope
    n = np.arange(128)[:, None]
    q = np.arange(SEQ)[None, :]
    c["cmpmask"] = ((16 * n + 31) <= q).astype(np.float32)
    k = np.arange(128)[:, None]
    qq = np.arange(128)[None, :]
    tri = np.zeros((128, 2, 128), np.float32)
    tri[:, 0] = np.where(k <= qq, 0.0, NEG)
    tri[:, 1] = np.where(k > qq, 0.0, NEG)
    c["tri"] = tri
    c["efull"] = (np.arange(32)[:, None] == (np.arange(SEQ)[None, :] // 64)).astype(np.float32)
    t_idx = np.arange(SEQ)
    cur = t_idx // 64
    jb = np.arange(32)[None, :]
    A = np.ones((SEQ, 32), np.float32)
    Bm = np.zeros((SEQ, 32), np.float32)
    forced0 = (jb == 0)
    forcedc = (jb == cur[:, None])
    forcedp = (jb == cur[:, None] - 1)
    fut = jb > cur[:, None]
    A[np.broadcast_to(forced0, A.shape) | forcedc | forcedp | fut] = 0.0
    Bm = np.where(np.broadcast_to(forced0, A.shape), 8192.0, Bm)
    Bm = np.where(forcedp, 8256.0, Bm)
    Bm = np.where(forcedc, 8320.0, Bm)
    Bm = np.where(fut, -8192.0 - 64.0 * jb, Bm).astype(np.float32)
    frc = np.zeros((128, 2, NT, 32), np.float32)
    frc[:, 0] = A.reshape(NT, 128, 32).transpose(1, 0, 2)
    frc[:, 1] = Bm.reshape(NT, 128, 32).transpose(1, 0, 2)
    c["force"] = frc
    nn = np.arange(128)
    c_lo = 16 * nn[:, None]
    s_lo = 64 * np.arange(32)[None, :]
    ov = np.clip(np.minimum(c_lo + 32, s_lo + 64) - np.maximum(c_lo, s_lo), 0, None)
    c["c2s"] = (ov.astype(np.float32) / 32.0)
    tp = np.arange(128)[:, None]
    tt = np.arange(128)[None, :]
    gl = np.zeros((128, 3, 128), np.float32)
    gl[:, 0] = (tp <= tt)
    gl[:, 1] = (tp > tt)
    gl[:, 2] = (tt >= tp)
    c["glamat"] = gl
    band = np.zeros((128, 3, 4, 128), np.float32)
    for gi, w in enumerate((2, 4, 8, 16)):
        for tok in range(128):
            cnt0 = min(tok + 1, w)
            for t_ in range(max(0, tok - w + 1), tok + 1):
                band[t_, 0, gi, tok] += 1.0 / cnt0
            band[tok, 0, gi, tok] -= 1.0
            for d_ in range(w):
                src = tok - d_
                if src >= 0:
                    band[src, 1, gi, tok] += 1.0 / w
                else:
                    band[128 + src, 2, gi, tok] += 1.0 / w
            band[tok, 1, gi, tok] -= 1.0
    c["band"] = band
    cv = np.zeros((128, 8), np.float32)
    cv[:, 0] = LN_EPS
    cv[:, 1] = RMS_EPS
    cv[:, 2] = 1.0
    c["cvec"] = cv
    return c


CONST_SHAPES = {
    "ident": [128, 128], "rope": [128, 4, NT, 8], "cmpmask": [128, SEQ], "tri": [128, 2, 128],
    "efull": [32, SEQ], "force": [128, 2, NT, 32], "c2s": [128, 32], "glamat": [128, 3, 128],
    "band": [128, 3, 4, 128], "cvec": [128, 8],
}

WEIGHT_SHAPES = {
    "w_in": [DEPTH, D, P_IN], "cmp_pos": [DEPTH, 2, 32, 64], "cmp_w1": [DEPTH, 2, 2048, 64],
    "cmp_w2": [DEPTH, 2, 64, 64], "gla_w_gate2": [DEPTH, 16, 192], "gla_b_gate": [DEPTH, 192],
    "gla_norm_g": [DEPTH, 384], "pool_w": [DEPTH, 4, 64, 64], "pool_scale": [DEPTH, 256],
    "w_out": [DEPTH, D, D], "ln1_g": [DEPTH, D], "ln1_b": [DEPTH, D], "w_up": [DEPTH, D, D_FF],
    "w_down": [DEPTH, D_FF, D], "ln2_g": [DEPTH, D], "ln2_b": [DEPTH, D],
}


class Builder:
    def __init__(self, n_seq=2, layers=(0, 1, 2, 3), phases=("nsa", "gla", "pool", "out", "ffn"), dump=None):
        self.n_seq = n_seq
        self.layers = list(layers)
        self.phases = phases
        self.dump = dump
        nc = self.nc = bass.Bass("TRN2", target_bir_lowering=False)
        es = self.es = ExitStack()
        S = self.S = Sched(nc, es)
        S._dcnt = {}
        self.x_d = Tile(nc.dram_tensor("x", [n_seq, SEQ, D], F32, kind="ExternalInput").ap(), "x")
        self.out_d = [S.attach_dma_sem(Tile(nc.dram_tensor("out", [n_seq, SEQ, D], F32, kind="ExternalOutput").ap(), "out"))]
        self.w = {k: Tile(nc.dram_tensor(k, shp, F32, kind="ExternalInput").ap(), k) for k, shp in WEIGHT_SHAPES.items()}
        self.c = {k: Tile(nc.dram_tensor("c_" + k, shp, F32, kind="ExternalInput").ap(), "c_" + k) for k, shp in CONST_SHAPES.items()}
        self.dump_d = {}
        self.h = self.sb("h", [128, NT, D], F32)
        self.h_t = [S.attach_dma_sem(Tile(self.h[:, t, :], f"h{t}")) for t in range(NT)]
        self.hT = self.sb("hT", [128, KC, SEQ], BF16)
        self.hT_t = [Tile(self.hT[:, :, t * 128:(t + 1) * 128], f"hT{t}") for t in range(NT)]
        self.mT = self.sb("mT", [128, KC, SEQ], BF16)
        self.mT_t = [Tile(self.mT[:, :, t * 128:(t + 1) * 128], f"mT{t}") for t in range(NT)]
        self.wbuf = [S.attach_dma_sem(Tile(self.sb(f"wb{i}", [128, KC, 512], BF16), f"wb{i}")) for i in range(3)]
        self.wrr = 0
        self.identb = S.attach_dma_sem(Tile(self.sb("identb", [128, 128], BF16), "identb"))
        self.cvec = S.attach_dma_sem(Tile(self.sb("cvec", [128, 8], F32), "cvec"))
        self.PS = es.enter_context(nc.psum_tensor("PS", [128, 4096], F32))
        self.bank = [Tile(self.PS[:, i * 512:(i + 1) * 512], f"bank{i}", ps=True) for i in range(8)]
        self.brr = 0
        S.dma("pool", self.identb, self.c["ident"], self.identb.ap, self.c["ident"].ap)
        S.dma("sp", self.cvec, self.c["cvec"], self.cvec.ap, self.c["cvec"].ap)

    _uid = 0

    def sb(self, name, shape, dt, es=None):
        self._uid += 1
        return (es or self.es).enter_context(self.nc.sbuf_tensor(f"{name}_{self._uid}", shape, dt))[:]

    def sbt(self, name, shape, dt, es=None, dma=False):
        t = Tile(self.sb(name, shape, dt, es), name)
        if dma:
            self.S.attach_dma_sem(t, "d_" + name)
        return t

    marks = None

    def mark(self, name):
        if self.marks is not None:
            self.marks.append((name, self.S.nops))

    def next_bank(self):
        b = self.bank[self.brr % 8]
        self.brr += 1
        return b

    def load_w(self, src_tile, src_ap, ncols):
        wb = self.wbuf[self.wrr % 3]
        self.wrr += 1
        self.S.dma("pool", wb, src_tile, wb.ap[:, :, 0:ncols], src_ap)
        return wb

    def dump_sb(self, name, tile, ap, shape, dt=F32):
        if self.dump is None or name not in self.dump:
            return
        S = self.S
        if name not in self.dump_d:
            self.dump_d[name] = S.attach_dma_sem(Tile(self.nc.dram_tensor("dbg_" + name, shape, dt, kind="ExternalOutput").ap(), "dbg_" + name))
        return self.dump_d[name]

    def build(self):
        S = self.S
        for s in range(self.n_seq):
            for t in range(NT):
                S.dma("sp", self.h_t[t], self.x_d, self.h_t[t].ap, self.x_d.ap[s, t * 128:(t + 1) * 128, :])
            with ExitStack() as es0:
                self.alloc_hb(es0)
                for t in range(NT):
                    self.make_hT(t)
                S.barrier()
            for l in self.layers:
                self.layer(l, s)
            if self.dump and "mT" in self.dump and s == 0:
                dd = S.attach_dma_sem(Tile(self.nc.dram_tensor("dbg_mT", [128, KC, SEQ], BF16, kind="ExternalOutput").ap(), "dbg_mT"))
                self.dump_d["mT"] = dd
                allm = Tile(self.mT, "mT_all")
                S.barrier()
                S.dma("sp", dd, allm, dd.ap, self.mT)
            for t in range(NT):
                S.dma("sp", self.out_d[0], self.h_t[t], self.out_d[0].ap[s, t * 128:(t + 1) * 128, :], self.h_t[t].ap)
        S.wait_tile("sp", self.out_d[0])
        for dd in self.dump_d.values():
            S.wait_tile("sp", dd)
        S.barrier()
        self.es.close()
        return self.nc

    def alloc_hb(self, es):
        self._hb = [Tile(self.sb(f"hb{i}", [128, D], BF16, es), f"hb{i}") for i in range(3)]
        self._ln = [dict(st=Tile(self.sb(f"lnst{i}", [128, 2, 6], F32, es)), mv=Tile(self.sb(f"lnmv{i}", [128, 2], F32, es)),
                         rs=Tile(self.sb(f"lnrs{i}", [128, 2], F32, es))) for i in range(8)]

    def make_hT(self, t):
        S, nc = self.S, self.nc
        for _ in self.make_hT_gen(t):
            pass

    def make_hT_gen(self, t):
        S, nc = self.S, self.nc
        hb = self._hb[t % 3]
        ht = self.h_t[t]
        S.op("act", [ht], [hb], lambda: nc.scalar.copy(hb.ap, ht.ap))
        yield
        bk = self.next_bank()
        pb = bk.ap.bitcast(BF16)
        for kc in range(KC):
            S.op("pe", [hb, self.identb], [bk],
                 lambda kc=kc: nc.tensor.transpose(pb[:, kc * 128:(kc + 1) * 128], hb.ap[:, kc * 128:(kc + 1) * 128], self.identb.ap),
                 inc=(kc == KC - 1))
        dst = self.hT_t[t]
        S.op("dve", [bk], [dst], lambda: nc.vector.tensor_copy(dst.ap, pb.rearrange("p (k c) -> p k c", k=KC)))

    def layer(self, l, s):
        if "nsa" in self.phases:
            self.nsa_phase(l)
        if "gla" in self.phases:
            self.gla_phase(l)
        if "pool" in self.phases:
            self.pool_phase(l)
        if "out" in self.phases:
            self.outproj_ln1(l)
        if "ffn" in self.phases:
            self.ffn_ln2(l)

    def ln_tile(self, t, tabs):
        for _ in self.ln_gen(t, tabs):
            pass

    def ln_gen(self, t, tabs):
        S, nc = self.S, self.nc
        ht = self.h_t[t]
        b = self._ln[t % 8]
        st, mv, rs = b["st"], b["mv"], b["rs"]
        S.op("dve", [ht], [st], lambda: nc.vector.bn_stats(st.ap[:, 0, :], ht.ap[:, 0:512]))
        S.op("dve", [ht], [st], lambda: nc.vector.bn_stats(st.ap[:, 1, :], ht.ap[:, 512:1024]))
        S.op("dve", [st], [mv], lambda: nc.vector.bn_aggr(mv.ap, st.ap))
        S.op("act", [mv, self.cvec], [rs], lambda: nc.scalar.activation(out=rs.ap[:, 0:1], in_=mv.ap[:, 1:2], func=AF.Sqrt, bias=self.cvec.ap[:, 0:1], scale=1.0))
        yield
        S.op("dve", [rs], [rs], lambda: nc.vector.reciprocal(rs.ap[:, 0:1], rs.ap[:, 0:1]))
        S.op("dve", [rs, mv], [rs], lambda: nc.vector.scalar_tensor_tensor(out=rs.ap[:, 1:2], in0=mv.ap[:, 0:1], scalar=-1.0, in1=rs.ap[:, 0:1], op0=ALU.mult, op1=ALU.mult))
        S.op("act", [ht, rs], [ht], lambda: nc.scalar.activation(out=ht.ap, in_=ht.ap, func=AF.Identity, scale=rs.ap[:, 0:1], bias=rs.ap[:, 1:2]))
        yield
        S.op("dve", [ht, tabs[0]], [ht], lambda: nc.vector.tensor_tensor(out=ht.ap, in0=ht.ap, in1=tabs[0].ap, op=ALU.mult))
        S.op("dve", [ht, tabs[1]], [ht], lambda: nc.vector.tensor_tensor(out=ht.ap, in0=ht.ap, in1=tabs[1].ap, op=ALU.add))
        yield from self.make_hT_gen(t)

    def load_ln_tabs(self, es, gname, bname, l):
        S = self.S
        G = self.sbt("lnG", [128, D], F32, es, dma=True)
        Bt = self.sbt("lnB", [128, D], F32, es, dma=True)
        S.dma("sp", G, self.w[gname], G.ap, self.w[gname].ap[l:l + 1, :].partition_broadcast(128))
        S.dma("sp", Bt, self.w[bname], Bt.ap, self.w[bname].ap[l:l + 1, :].partition_broadcast(128))
        return G, Bt

    def outproj_ln1(self, l):
        S, nc = self.S, self.nc
        with ExitStack() as es:
            self.alloc_hb(es)
            tabs = self.load_ln_tabs(es, "ln1_g", "ln1_b", l)
            wo = self.w["w_out"]
            wts = [self.load_w(wo, wo.ap[l].rearrange("(k p) c -> p k c", p=128)[:, :, hf * 512:(hf + 1) * 512], 512) for hf in range(2)]
            def tile_gen(t):
                ht = self.h_t[t]
                bks = []
                for hf in range(2):
                    bk = self.next_bank()
                    bks.append(bk)
                    for kc in range(KC):
                        S.op("pe", [self.mT_t[t], wts[hf]], [bk],
                             lambda kc=kc, bk=bk, hf=hf: nc.tensor.matmul(bk.ap, self.mT[:, kc, t * 128:(t + 1) * 128], wts[hf].ap[:, kc, :], start=(kc == 0), stop=(kc == KC - 1)),
                             inc=(kc == KC - 1))
                yield
                for hf in range(2):
                    S.op("dve", [ht, bks[hf]], [ht],
                         lambda hf=hf: nc.vector.scalar_tensor_tensor(out=ht.ap[:, hf * 512:(hf + 1) * 512], in0=ht.ap[:, hf * 512:(hf + 1) * 512], scalar=ALPHA, in1=bks[hf].ap, op0=ALU.mult, op1=ALU.add))
                yield from self.ln_gen(t, tabs)

            run_pipeline(tile_gen(t) for t in range(NT))
            S.barrier()

    def ffn_ln2(self, l):
        S, nc = self.S, self.nc
        with ExitStack() as es:
            self.alloc_hb(es)
            tabs = self.load_ln_tabs(es, "ln2_g", "ln2_b", l)
            hid = self.sb("hidT", [128, KC, SEQ], BF16, es)
            hid_c = [[Tile(hid[:, c, tg * 512:(tg + 1) * 512], f"hid{c}_{tg}") for tg in range(4)] for c in range(KC)]
            rtmp = [Tile(self.sb(f"rtmp{i}", [128, 512], BF16, es), f"rtmp{i}") for i in range(3)]
            wu, wd = self.w["w_up"], self.w["w_down"]
            rr = 0
            for fg in range(4):
                for wt in range(2):
                    f0 = fg * 1024 + wt * 512
                    Wt = self.load_w(wu, wu.ap[l].rearrange("(k p) c -> p k c", p=128)[:, :, f0:f0 + 512], 512)
                    for fc in range(4):
                        c = wt * 4 + fc
                        for tg in range(4):
                            bk = self.next_bank()
                            for kc in range(KC):
                                S.op("pe", [self.hT_t[4 * tg + i] for i in range(4)] + [Wt], [bk],
                                     lambda kc=kc, bk=bk, fc=fc, tg=tg, Wt=Wt: nc.tensor.matmul(bk.ap, Wt.ap[:, kc, fc * 128:(fc + 1) * 128], self.hT[:, kc, tg * 512:(tg + 1) * 512], start=(kc == 0), stop=(kc == KC - 1)),
                                     inc=(kc == KC - 1))
                            rt = rtmp[rr % 3]
                            rr += 1
                            S.op("act", [bk], [rt], lambda bk=bk, rt=rt: nc.scalar.activation(out=rt.ap, in_=bk.ap, func=AF.Relu))
                            hc = hid_c[c][tg]
                            S.op("dve", [bk, rt], [hc], lambda bk=bk, rt=rt, hc=hc: nc.vector.tensor_tensor(out=hc.ap, in0=bk.ap, in1=rt.ap, op=ALU.mult))
                for hf in range(2):
                    Wt = self.load_w(wd, wd.ap[l, fg * 1024:(fg + 1) * 1024, :].rearrange("(k p) c -> p k c", p=128)[:, :, hf * 512:(hf + 1) * 512], 512)

                    def down_gen(t, hf=hf, Wt=Wt, fg=fg):
                        ht = self.h_t[t]
                        bk = self.next_bank()
                        for kc in range(KC):
                            S.op("pe", [hid_c[kc][t // 4], Wt], [bk],
                                 lambda kc=kc, bk=bk, t=t, Wt=Wt: nc.tensor.matmul(bk.ap, hid[:, kc, t * 128:(t + 1) * 128], Wt.ap[:, kc, :], start=(kc == 0), stop=(kc == KC - 1)),
                                 inc=(kc == KC - 1))
                        yield
                        sl = slice(hf * 512, (hf + 1) * 512)
                        if fg == 0:
                            S.op("dve", [ht, bk], [ht], lambda bk=bk, ht=ht, sl=sl: nc.vector.scalar_tensor_tensor(out=ht.ap[:, sl], in0=ht.ap[:, sl], scalar=ALPHA, in1=bk.ap, op0=ALU.mult, op1=ALU.add))
                        else:
                            S.op("dve", [ht, bk], [ht], lambda bk=bk, ht=ht, sl=sl: nc.vector.tensor_tensor(out=ht.ap[:, sl], in0=ht.ap[:, sl], in1=bk.ap, op=ALU.add))
                        if fg == 3 and hf == 1:
                            yield from self.ln_gen(t, tabs)

                    run_pipeline(down_gen(t) for t in range(NT))
            S.barrier()

    def pool_phase(self, l):
        S, nc = self.S, self.nc
        with ExitStack() as es:
            band = self.sbt("band", [128, 3, 4, 128], BF16, es, dma=True)
            S.dma("pool", band, self.c["band"], band.ap, self.c["band"].ap)
            wpb = self.sbt("wpb", [128, 2, 128], BF16, es, dma=True)
            S.op("dve", [], [wpb], lambda: nc.vector.memset(wpb.ap, 0.0))
            pw = self.w["pool_w"]
            for g in range(4):
                r0 = (g % 2) * 64
                S.dma("pool", wpb, pw, wpb.ap[r0:r0 + 64, g // 2, r0:r0 + 64], pw.ap[l, g])
            psc = self.sbt("psc", [128, 2], F32, es, dma=True)
            S.dma("sp", psc, self.w["pool_scale"], psc.ap, self.w["pool_scale"].ap[l].rearrange("(a p) -> p a", p=128), allow_slow_non_contiguous=True)
            u = [Tile(self.sb(f"pu{i}", [128, 256], BF16, es), f"pu{i}") for i in range(3)]
            pT = [Tile(self.sb(f"pT{i}", [128, 2, 128], BF16, es), f"pT{i}") for i in range(2)]
            wi = self.w["w_in"]
            Wt = self.load_w(wi, wi.ap[l].rearrange("(k p) c -> p k c", p=128)[:, :, 2338:2594], 256)
            def tile_gen(t):
                bk = self.next_bank()
                for kc in range(KC):
                    S.op("pe", [self.hT_t[t], Wt], [bk],
                         lambda kc=kc, bk=bk: nc.tensor.matmul(bk.ap[:, 0:256], self.hT[:, kc, t * 128:(t + 1) * 128], Wt.ap[:, kc, 0:256], start=(kc == 0), stop=(kc == KC - 1)),
                         inc=(kc == KC - 1))
                yield
                uc, up = u[t % 3], u[(t - 1) % 3]
                S.op("act", [bk], [uc], lambda bk=bk, uc=uc: nc.scalar.copy(uc.ap, bk.ap[:, 0:256]))
                b2 = self.next_bank()
                for g in range(4):
                    r0 = (g % 2) * 64
                    o = b2.ap[r0:r0 + 64, (g // 2) * 128:(g // 2 + 1) * 128]
                    if t == 0:
                        S.op("pe", [uc, band], [b2], lambda o=o, g=g, uc=uc: nc.tensor.matmul(o, uc.ap[:, g * 64:(g + 1) * 64], band.ap[:, 0, g, :], start=True, stop=True), inc=(g == 3))
                    else:
                        S.op("pe", [uc, band], [b2], lambda o=o, g=g, uc=uc: nc.tensor.matmul(o, uc.ap[:, g * 64:(g + 1) * 64], band.ap[:, 1, g, :], start=True, stop=False), inc=False)
                        S.op("pe", [up, band], [b2], lambda o=o, g=g, up=up: nc.tensor.matmul(o, up.ap[:, g * 64:(g + 1) * 64], band.ap[:, 2, g, :], start=False, stop=True), inc=(g == 3))
                yield
                pt = pT[t % 2]
                S.op("dve", [b2], [pt], lambda b2=b2, pt=pt: nc.vector.tensor_copy(pt.ap, b2.ap[:, 0:256].rearrange("p (a c) -> p a c", a=2)))
                b3 = self.next_bank()
                for a in range(2):
                    S.op("pe", [pt, wpb], [b3], lambda a=a, b3=b3, pt=pt: nc.tensor.matmul(b3.ap[:, a * 128:(a + 1) * 128], wpb.ap[:, a, :], pt.ap[:, a, :], start=True, stop=True), inc=(a == 1))
                yield
                for a in range(2):
                    S.op("act", [b3, psc], [self.mT_t[t]], lambda a=a, b3=b3: nc.scalar.activation(out=self.mT[:, 6 + a, t * 128:(t + 1) * 128], in_=b3.ap[:, a * 128:(a + 1) * 128], func=AF.Copy, scale=psc.ap[:, a:a + 1]))

            run_pipeline(tile_gen(t) for t in range(NT))
            S.barrier()

    def gla_phase(self, l):
        S, nc = self.S, self.nc
        NB = 3
        with ExitStack() as es:
            gm = self.sbt("glamat", [128, 3, 128], BF16, es, dma=True)
            S.dma("pool", gm, self.c["glamat"], gm.ap, self.c["glamat"].ap)
            cmask = self.sbt("gcm", [128, 128], BF16, es)
            S.op("dve", [gm], [cmask], lambda: nc.vector.tensor_copy(cmask.ap, gm.ap[:, 2, :]))
            ones = self.sbt("gones", [128, 1], BF16, es)
            S.op("dve", [], [ones], lambda: nc.vector.memset(ones.ap, 1.0))
            wg = self.sbt("wg2", [17, 192], BF16, es, dma=True)
            S.dma("pool", wg, self.w["gla_w_gate2"], wg.ap[0:16, :], self.w["gla_w_gate2"].ap[l])
            S.dma("pool", wg, self.w["gla_b_gate"], wg.ap[16:17, :], self.w["gla_b_gate"].ap[l:l + 1, :])
            ngt = self.sbt("ngt", [128, 384], F32, es, dma=True)
            S.dma("sp", ngt, self.w["gla_norm_g"], ngt.ap, self.w["gla_norm_g"].ap[l:l + 1, :].partition_broadcast(128))
            St = self.sbt("gS", [64, 6, 64], F32, es)
            Sbs = [self.sbt(f"gSb{i}", [64, 6, 64], BF16, es) for i in range(2)]
            S.op("dve", [], [St], lambda: nc.vector.memset(St.ap, 0.0))
            for sb_ in Sbs:
                S.op("dve", [], [sb_], lambda sb_=sb_: nc.vector.memset(sb_.ap, 0.0))
            stmp = self.sbt("gstmp", [64, 6, 64], F32, es)

            def bufs(name, shape, dt):
                return [self.sbt(f"{name}{i}", shape, dt, es) for i in range(NB)]

            glrTs = bufs("glrT", [17, 128], BF16)
            for g_ in glrTs:
                S.op("dve", [], [g_], lambda g_=g_: nc.vector.memset(g_.ap, 1.0))
            glrs = bufs("glr", [128, 16], BF16)
            lsgs = bufs("lsg", [128, 192], F32)
            lhis = bufs("lsghi", [128, 192], BF16)
            llos = bufs("lsglo", [128, 192], BF16)
            exs = bufs("gex", [128, 3, 192], F32)
            qds = bufs("gqd", [128, 192], BF16)
            kis = bufs("gki", [128, 192], BF16)
            kes = bufs("gke", [128, 192], BF16)
            vts = bufs("gvt", [128, 384], BF16)
            qkTs = bufs("gqkT", [64, 6, 128], BF16)
            Ams = bufs("gAm", [128, 6, 128], BF16)
            decs = bufs("gdec", [64, 6], F32)
            sqs = bufs("gsq", [128, 384], F32)
            sss = bufs("gss", [128, 6], F32)
            sils = [self.sbt(f"gsil{i}", [128, 384], F32, es) for i in range(4)]
            gos = bufs("gout", [128, 384], BF16)
            qks = bufs("gqk", [128, 384], F32)
            wi = self.w["w_in"]
            wv = wi.ap[l].rearrange("(k p) c -> p k c", p=128)
            gcols = ((1170, 384), (1554, 400), (1954, 384))
            Ws = [self.load_w(wi, wv[:, :, c0:c0 + cw], cw) for (c0, cw) in gcols]
            PG = Tile(self.PS[:, 0:1536], "PG", ps=True)
            PB = Tile(self.PS[:, 1536:2048], "PB", ps=True)
            PA = Tile(self.PS[:, 2048:3072], "PA", ps=True)
            PO = Tile(self.PS[:, 3072:3584], "PO", ps=True)
            PD = Tile(self.PS[:, 3584:4096], "PD", ps=True)
            S.barrier()

            def tile_gen(t):
                i3 = t % NB
                glrT, glr, lsg, lhi, llo, ex = glrTs[i3], glrs[i3], lsgs[i3], lhis[i3], llos[i3], exs[i3]
                qd, ki, ke, vt, qkT, Am, dec = qds[i3], kis[i3], kes[i3], vts[i3], qkTs[i3], Ams[i3], decs[i3]
                sq, ss, sil, go, qk = sqs[i3], sss[i3], sils[t % 4], gos[i3], qks[i3]
                o1 = sq
                Sb_in, Sb_out = Sbs[(t + 1) % 2], Sbs[t % 2]
                for i, (c0_, cw) in enumerate(gcols):
                    for kc in range(KC):
                        S.op("pe", [self.hT_t[t], Ws[i]], [PG],
                             lambda kc=kc, i=i, cw=cw: nc.tensor.matmul(PG.ap[:, i * 512:i * 512 + cw], self.hT[:, kc, t * 128:(t + 1) * 128], Ws[i].ap[:, kc, 0:cw], start=(kc == 0), stop=(kc == KC - 1)),
                             inc=(kc == KC - 1 and i == 2))
                g_q, g_k, g_v, g_lr, g_og = PG.ap[:, 0:192], PG.ap[:, 192:384], PG.ap[:, 512:896], PG.ap[:, 896:912], PG.ap[:, 1024:1408]
                yield
                S.op("act", [PG], [glr], lambda: nc.scalar.copy(glr.ap, g_lr))
                S.op("act", [PG], [qk], lambda: nc.scalar.copy(qk.ap, PG.ap[:, 0:384]))
                S.op("act", [PG], [vt], lambda: nc.scalar.copy(vt.ap, g_v))
                S.op("act", [PG], [sil], lambda: nc.scalar.activation(out=sil.ap, in_=g_og, func=AF.Silu))
                pbb = PB.ap.bitcast(BF16)
                S.op("pe", [glr, self.identb], [PB], lambda: nc.tensor.transpose(pbb[0:16, 832:960], glr.ap, self.identb.ap))
                yield
                S.op("dve", [PB], [glrT], lambda: nc.vector.tensor_copy(glrT.ap[0:16, :], pbb[0:16, 832:960]))
                S.op("pe", [glrT, wg], [PB], lambda: nc.tensor.matmul(PB.ap[:, 0:192], glrT.ap, wg.ap, start=True, stop=True))
                yield
                S.op("act", [PB], [lsg], lambda: nc.scalar.activation(out=lsg.ap, in_=PB.ap[:, 0:192], func=AF.Exp, scale=-1.0))
                S.op("act", [lsg, self.cvec], [lsg], lambda: nc.scalar.activation(out=lsg.ap, in_=lsg.ap, func=AF.Ln, bias=self.cvec.ap[:, 2:3], scale=1.0))
                yield
                S.op("dve", [lsg], [lhi], lambda: nc.vector.tensor_copy(lhi.ap, lsg.ap))
                S.op("dve", [lsg, lhi], [llo], lambda: nc.vector.tensor_tensor(out=llo.ap, in0=lsg.ap, in1=lhi.ap, op=ALU.subtract))
                for m_ in range(2):
                    S.op("pe", [lhi, gm], [PB], lambda m_=m_: nc.tensor.matmul(PB.ap[:, m_ * 192:(m_ + 1) * 192], gm.ap[:, m_, :], lhi.ap, start=True, stop=False), inc=False)
                    S.op("pe", [llo, gm], [PB], lambda m_=m_: nc.tensor.matmul(PB.ap[:, m_ * 192:(m_ + 1) * 192], gm.ap[:, m_, :], llo.ap, start=False, stop=True), inc=False)
                for hh in range(6):
                    po_ = PB.ap[(hh % 2) * 32:(hh % 2) * 32 + 32, 384 + hh:385 + hh]
                    S.op("pe", [lhi, ones], [PB], lambda hh=hh, po_=po_: nc.tensor.matmul(po_, lhi.ap[:, hh * 32:(hh + 1) * 32], ones.ap, start=True, stop=False), inc=False)
                    S.op("pe", [llo, ones], [PB], lambda hh=hh, po_=po_: nc.tensor.matmul(po_, llo.ap[:, hh * 32:(hh + 1) * 32], ones.ap, start=False, stop=True), inc=(hh == 5))
                yield
                S.op("act", [PB], [ex], lambda: nc.scalar.activation(out=ex.ap[:, 0, :], in_=PB.ap[:, 0:192], func=AF.Exp, scale=-1.0 / 16))
                S.op("act", [PB], [ex], lambda: nc.scalar.activation(out=ex.ap[:, 1, :], in_=PB.ap[:, 0:192], func=AF.Exp, scale=1.0 / 16))
                S.op("act", [PB], [ex], lambda: nc.scalar.activation(out=ex.ap[:, 2, :], in_=PB.ap[:, 192:384], func=AF.Exp, scale=-1.0 / 16))
                for par in range(2):
                    S.op("act", [PB], [dec], lambda par=par: nc.scalar.activation(out=dec.ap[par * 32:par * 32 + 32, par::2], in_=PB.ap[par * 32:par * 32 + 32, 384 + par:390:2], func=AF.Exp, scale=-1.0 / 16))
                yield
                S.op("dve", [qk, ex], [qd], lambda: nc.vector.scalar_tensor_tensor(out=qd.ap, in0=qk.ap[:, 0:192], scalar=32 ** -0.5, in1=ex.ap[:, 0, :], op0=ALU.mult, op1=ALU.mult))
                S.op("dve", [qk, ex], [ki], lambda: nc.vector.tensor_tensor(out=ki.ap, in0=qk.ap[:, 192:384], in1=ex.ap[:, 1, :], op=ALU.mult))
                S.op("dve", [qk, ex], [ke], lambda: nc.vector.tensor_tensor(out=ke.ap, in0=qk.ap[:, 192:384], in1=ex.ap[:, 2, :], op=ALU.mult))
                pab = PA.ap[:, 512:1024].bitcast(BF16)
                for j in range(3):
                    S.op("pe", [qd, self.identb], [PA], lambda j=j: nc.tensor.transpose(pab[0:64, j * 128:(j + 1) * 128], qd.ap[:, j * 64:(j + 1) * 64], self.identb.ap), inc=False)
                for j in range(3):
                    S.op("pe", [ki, self.identb], [PA], lambda j=j: nc.tensor.transpose(pab[0:64, (3 + j) * 128:(4 + j) * 128], ki.ap[:, j * 64:(j + 1) * 64], self.identb.ap), inc=(j == 2))
                for hh in range(6):
                    S.op("pe", [ke, vt], [PD], lambda hh=hh: nc.tensor.matmul(PD.ap[(hh % 2) * 32:(hh % 2) * 32 + 32, hh * 64:(hh + 1) * 64], ke.ap[:, hh * 32:(hh + 1) * 32], vt.ap[:, hh * 64:(hh + 1) * 64], start=True, stop=True), inc=(hh == 5))
                yield
                S.op("dve", [PA], [qkT], lambda: nc.vector.tensor_copy(qkT.ap, pab[0:64, 0:768].rearrange("p (a c) -> p a c", a=6)))
                for par in range(2):
                    rs_ = slice(par * 32, par * 32 + 32)
                    S.op("dve", [St, dec], [stmp], lambda par=par, rs_=rs_: nc.vector.tensor_tensor(out=stmp.ap[rs_, par::2, :], in0=St.ap[rs_, par::2, :], in1=dec.ap[rs_, par::2].unsqueeze(2).to_broadcast([32, 3, 64]), op=ALU.mult))
                    S.op("dve", [stmp, PD], [St], lambda par=par, rs_=rs_: nc.vector.tensor_tensor(out=St.ap[rs_, par::2, :], in0=stmp.ap[rs_, par::2, :], in1=PD.ap[rs_, 0:384].rearrange("p (a c) -> p a c", a=6)[:, par::2, :], op=ALU.add))
                for hh in range(6):
                    r0 = (hh % 2) * 32
                    pc0 = (hh % 2) * 512 + (hh // 2) * 128
                    S.op("pe", [qkT], [PA], lambda hh=hh, r0=r0, pc0=pc0: nc.tensor.matmul(PA.ap[:, pc0:pc0 + 128], qkT.ap[r0:r0 + 32, 3 + hh // 2, :], qkT.ap[r0:r0 + 32, hh // 2, :], start=True, stop=True), inc=(hh == 5))
                yield
                for par in range(2):
                    S.op("act", [St], [Sb_out], lambda par=par: nc.scalar.copy(Sb_out.ap[par * 32:par * 32 + 32, par::2, :], St.ap[par * 32:par * 32 + 32, par::2, :]))
                for par in range(2):
                    S.op("dve", [PA, cmask], [Am], lambda par=par: nc.vector.tensor_tensor(out=Am.ap[:, par::2, :], in0=PA.ap[:, par * 512:par * 512 + 384].rearrange("p (a c) -> p a c", a=3), in1=cmask.ap.unsqueeze(1).to_broadcast([128, 3, 128]), op=ALU.mult))
                for hh in range(6):
                    r0 = (hh % 2) * 32
                    S.op("pe", [Am, vt], [PO], lambda hh=hh: nc.tensor.matmul(PO.ap[:, hh * 64:(hh + 1) * 64], Am.ap[:, hh, :], vt.ap[:, hh * 64:(hh + 1) * 64], start=True, stop=False), inc=False)
                    S.op("pe", [qkT, Sb_in], [PO], lambda hh=hh, r0=r0: nc.tensor.matmul(PO.ap[:, hh * 64:(hh + 1) * 64], qkT.ap[r0:r0 + 32, hh // 2, :], Sb_in.ap[r0:r0 + 32, hh, :], start=False, stop=True), inc=(hh == 5))
                yield
                S.op("act", [PO], [sq], lambda: nc.scalar.activation(out=sq.ap, in_=PO.ap[:, 0:384], func=AF.Square))
                yield
                S.op("dve", [sq], [ss], lambda: nc.vector.tensor_reduce(out=ss.ap, in_=sq.ap.rearrange("p (a c) -> p a c", a=6), axis=AX.X, op=ALU.add))
                S.op("act", [ss, self.cvec], [ss], lambda: nc.scalar.activation(out=ss.ap, in_=ss.ap, func=AF.Sqrt, bias=self.cvec.ap[:, 1:2], scale=1.0 / 64))
                yield
                S.op("dve", [ss], [ss], lambda: nc.vector.reciprocal(ss.ap, ss.ap))
                S.op("dve", [PO, ss], [o1], lambda: nc.vector.tensor_tensor(out=o1.ap.rearrange("p (a c) -> p a c", a=6), in0=PO.ap[:, 0:384].rearrange("p (a c) -> p a c", a=6), in1=ss.ap.unsqueeze(2).to_broadcast([128, 6, 64]), op=ALU.mult))
                S.op("dve", [o1, ngt], [o1], lambda: nc.vector.tensor_tensor(out=o1.ap, in0=o1.ap, in1=ngt.ap, op=ALU.mult))
                S.op("dve", [o1, sil], [go], lambda: nc.vector.tensor_tensor(out=go.ap, in0=o1.ap, in1=sil.ap, op=ALU.mult))
                pdb = PD.ap.bitcast(BF16)
                for j in range(3):
                    S.op("pe", [go, self.identb], [PD], lambda j=j: nc.tensor.transpose(pdb[:, 384 + j * 128:384 + (j + 1) * 128], go.ap[:, j * 128:(j + 1) * 128], self.identb.ap), inc=(j == 2))
                yield
                S.op("act", [PD], [self.mT_t[t]], lambda: nc.scalar.copy(self.mT[:, 3:6, t * 128:(t + 1) * 128], pdb[:, 384:768].rearrange("p (a c) -> p a c", a=3)))

            run_pipeline((tile_gen(t) for t in range(NT)), start_every=3)
            S.barrier()

    def nsa_phase(self, l):
        S, nc = self.S, self.nc
        with ExitStack() as es:
            qT = self.sb("qT", [128, 3, SEQ], BF16, es)
            qT_g = [Tile(qT[:, :, qg * 512:(qg + 1) * 512], f"qT{qg}") for qg in range(4)]
            kT = self.sb("kT12", [128, 2, SEQ], BF16, es)
            kT_b = [None] + [Tile(kT[:, br, :], f"kT{br + 1}") for br in range(2)]
            Va = self.sb("Vaug", [128, NT * 4, 65], BF16, es)
            Va_t = [Tile(Va[:, t * 4:(t + 1) * 4, :], f"Va{t}") for t in range(NT)]
            Va_all = Tile(Va, "Va_all")
            gates = self.sbt("gates", [128, NT, 18], F32, es)
            S.op("dve", [], [Va_all], lambda: nc.vector.memset(Va[:, :, 64:65], 1.0))
            for t in range(NT):
                Va_t[t].lw = Va_all.lw
            kcT2 = [self.sbt(f"kcT2_{g}", [128, 128], BF16, es) for g in range(2)]
            vca = [self.sbt(f"vca_{g}", [128, 97], BF16, es, dma=True) for g in range(2)]
            selTt = self.sb("selT", [64, SEQ], BF16, es)
            selT = [Tile(selTt[g * 32:(g + 1) * 32, :], f"selT{g}") for g in range(2)]
            with ExitStack() as es1:
                self.nsa_inproj(l, es1, qT, qT_g, kT, kT_b, Va, Va_t, gates, kcT2, vca)
            S.barrier()
            with ExitStack() as es2:
                self.nsa_attend(l, es2, qT, qT_g, kT, kT_b, Va, Va_t, gates, kcT2, vca, selT)
            S.barrier()

    def nsa_inproj(self, l, es, qT, qT_g, kT, kT_b, Va, Va_t, gates, kcT2, vca):
        S, nc = self.S, self.nc
        v0T = self.sbt("v0T", [128, SEQ], BF16, es)
        k0T = self.sbt("k0T", [128, SEQ], BF16, es)
        kT_b = [k0T] + kT_b[1:]
        es_outer = es
        es = es_outer.enter_context(ExitStack())
        rope = self.sbt("rope", [128, 4, NT, 8], F32, es, dma=True)
        S.dma("sp", rope, self.c["rope"], rope.ap, self.c["rope"].ap)
        qtok = [self.sbt(f"qtok{i}", [128, 384], BF16, es) for i in range(2)]
        ktok = [self.sbt(f"ktok{i}", [128, 3, 128], BF16, es) for i in range(2)]
        rt = [self.sbt(f"ropet{i}", [128, 4, 6, 8], F32, es) for i in range(2)]
        wi_ = self.w["w_in"]
        wv = wi_.ap[l].rearrange("(k p) c -> p k c", p=128)

        def bc(tab, t, shp):
            a = rope.ap[:, tab, t, :]
            for _ in range(len(shp) - 2):
                a = a.unsqueeze(1)
            return a.to_broadcast(shp)

        def do_rope(t, x, out, tabc, tabs, shp, tmp, reads, outt):
            pre = (slice(None),) * (len(shp) - 1)
            x1, x2 = x[pre + (slice(0, 8),)], x[pre + (slice(8, 16),)]
            out1, out2 = out[pre + (slice(0, 8),)], out[pre + (slice(8, 16),)]
            n = shp[1] * (shp[2] if len(shp) == 4 else 1)
            tv = [tmp.ap[:, i, 0:n, :] for i in range(4)]
            if len(shp) == 4:
                tv = [a.rearrange("p (a b) c -> p a b c", a=shp[1]) for a in tv]
            S.op("dve", reads + [rope, outt], [tmp], lambda: nc.vector.tensor_tensor(out=tv[0], in0=x1, in1=bc(tabc, t, shp), op=ALU.mult))
            S.op("dve", reads + [rope], [tmp], lambda: nc.vector.tensor_tensor(out=tv[1], in0=x2, in1=bc(tabs, t, shp), op=ALU.mult))
            S.op("dve", reads + [rope], [tmp], lambda: nc.vector.tensor_tensor(out=tv[2], in0=x1, in1=bc(tabs, t, shp), op=ALU.mult))
            S.op("dve", reads + [rope], [tmp], lambda: nc.vector.tensor_tensor(out=tv[3], in0=x2, in1=bc(tabc, t, shp), op=ALU.mult))
            S.op("dve", [tmp], [outt], lambda: nc.vector.tensor_tensor(out=out1, in0=tv[0], in1=tv[1], op=ALU.subtract))
            S.op("dve", [tmp], [outt], lambda: nc.vector.tensor_tensor(out=out2, in0=tv[2], in1=tv[3], op=ALU.add))

        for wi, (c0, cw) in enumerate(((0, 512), (512, 512), (1024, 146))):
            Wt = self.load_w(wi_, wv[:, :, c0:c0 + cw], cw)
            pend = None
            for t in range(NT):
                bk = self.next_bank()
                for kc in range(KC):
                    S.op("pe", [self.hT_t[t], Wt], [bk],
                         lambda kc=kc, bk=bk, t=t: nc.tensor.matmul(bk.ap[:, 0:cw], self.hT[:, kc, t * 128:(t + 1) * 128], Wt.ap[:, kc, 0:cw], start=(kc == 0), stop=(kc == KC - 1)),
                         inc=(kc == KC - 1))
                tsl = slice(t * 128, (t + 1) * 128)
                if wi == 0:
                    qt_, kt_, tmp = qtok[t % 2], ktok[t % 2], rt[t % 2]
                    qsrc = bk.ap[:, 0:384].rearrange("p (a d) -> p a d", a=6)
                    qdst = qt_.ap.rearrange("p (a d) -> p a d", a=6)
                    S.op("act", [bk], [qt_], lambda bk=bk, qt_=qt_: nc.scalar.activation(out=qt_.ap, in_=bk.ap[:, 0:384], func=AF.Copy, scale=0.125))
                    do_rope(t, qsrc, qdst, 0, 1, [128, 6, 8], tmp, [bk], qt_)
                    ksrc = bk.ap[:, 384:512].rearrange("p (g d) -> p g d", g=2)
                    kdst = kt_.ap[:, 0, :].rearrange("p (g d) -> p g d", g=2)
                    S.op("act", [bk], [kt_], lambda ksrc=ksrc, kdst=kdst: nc.scalar.copy(kdst, ksrc))
                    do_rope(t, ksrc, kdst, 2, 3, [128, 2, 8], tmp, [bk], kt_)

                    def tr(t=t, qt_=qt_, kt_=kt_, tsl=tsl):
                        tb_ = self.next_bank()
                        pb = tb_.ap.bitcast(BF16)
                        for hh in range(6):
                            g_, h_ = hh // 3, hh % 3
                            S.op("pe", [qt_, self.identb], [tb_], lambda hh=hh, g_=g_, h_=h_: nc.tensor.transpose(pb[g_ * 64:(g_ + 1) * 64, h_ * 128:(h_ + 1) * 128], qt_.ap[:, hh * 64:(hh + 1) * 64], self.identb.ap), inc=False)
                        S.op("pe", [kt_, self.identb], [tb_], lambda: nc.tensor.transpose(pb[:, 384:512], kt_.ap[:, 0, :], self.identb.ap))
                        S.op("act", [tb_], [qT_g[t // 4]], lambda: nc.scalar.copy(qT[:, :, tsl], pb[:, 0:384].rearrange("p (a c) -> p a c", a=3)))
                        S.op("act", [tb_], [kT_b[0]], lambda: nc.scalar.copy(k0T.ap[:, tsl], pb[:, 384:512]))
                elif wi == 1:
                    kt_, tmp = ktok[t % 2], rt[t % 2]
                    S.op("act", [bk], [kt_], lambda bk=bk, kt_=kt_: nc.scalar.copy(kt_.ap[:, 0, :], bk.ap[:, 0:128]))
                    for b_ in range(2):
                        ksrc = bk.ap[:, 128 + 256 * b_:256 + 256 * b_].rearrange("p (g d) -> p g d", g=2)
                        kdst = kt_.ap[:, 1 + b_, :].rearrange("p (g d) -> p g d", g=2)
                        S.op("act", [bk], [kt_], lambda ksrc=ksrc, kdst=kdst: nc.scalar.copy(kdst, ksrc))
                        do_rope(t, ksrc, kdst, 2, 3, [128, 2, 8], tmp, [bk], kt_)
                    S.op("act", [bk], [Va_t[t]], lambda bk=bk, t=t: nc.scalar.copy(Va[:, t * 4:t * 4 + 2, 0:64], bk.ap[:, 256:384].rearrange("p (g d) -> p g d", g=2)))

                    def tr(t=t, kt_=kt_, tsl=tsl):
                        tb_ = self.next_bank()
                        pb = tb_.ap.bitcast(BF16)
                        for j in range(3):
                            S.op("pe", [kt_, self.identb], [tb_], lambda j=j: nc.tensor.transpose(pb[:, j * 128:(j + 1) * 128], kt_.ap[:, j, :], self.identb.ap), inc=(j == 2))
                        S.op("act", [tb_], [v0T], lambda: nc.scalar.copy(v0T.ap[:, tsl], pb[:, 0:128]))
                        S.op("act", [tb_], [kT_b[1]], lambda: nc.scalar.copy(kT[:, 0, tsl], pb[:, 128:256]))
                        S.op("act", [tb_], [kT_b[2]], lambda: nc.scalar.copy(kT[:, 1, tsl], pb[:, 256:384]))
                else:
                    S.op("act", [bk], [Va_t[t]], lambda bk=bk, t=t: nc.scalar.copy(Va[:, t * 4 + 2:t * 4 + 4, 0:64], bk.ap[:, 0:128].rearrange("p (g d) -> p g d", g=2)))
                    S.op("act", [bk], [gates], lambda bk=bk, t=t: nc.scalar.activation(out=gates.ap[:, t, :], in_=bk.ap[:, 128:146], func=AF.Sigmoid))
                    tr = None
                if pend is not None:
                    pend()
                pend = tr
            if pend is not None:
                pend()
        S.barrier()
        es.close()
        es = es_outer
        self.mark("compress")
        w1sb = self.sbt("w1sb", [128, 2, 32, 64], BF16, es, dma=True)
        cw1 = self.w["cmp_w1"]
        for kv in range(2):
            for hf in range(2):
                S.dma("pool", w1sb, cw1, w1sb.ap[hf * 64:(hf + 1) * 64, kv], cw1.ap[l, kv].rearrange("(j d) h -> d j h", d=64))
        possb = self.sbt("possb", [32, 2, 64], BF16, es, dma=True)
        S.dma("pool", possb, self.w["cmp_pos"], possb.ap, self.w["cmp_pos"].ap[l].rearrange("k j d -> j k d"))
        w2sb = self.sbt("w2sb", [64, 3, 64], BF16, es, dma=True)
        cw2 = self.w["cmp_w2"]
        S.dma("pool", w2sb, cw2, w2sb.ap[:, 0, :], cw2.ap[l, 0])
        S.dma("pool", w2sb, cw2, w2sb.ap[:, 1, :], cw2.ap[l, 0])
        S.dma("pool", w2sb, cw2, w2sb.ap[:, 2, :], cw2.ap[l, 1])
        for g in range(2):
            S.dma("pool", vca[g], self.c["c2s"], vca[g].ap[:, 65:97], self.c["c2s"].ap)
            S.op("dve", [], [vca[g]], lambda g=g: nc.vector.memset(vca[g].ap[:, 64:65], 1.0))
        posT = self.sbt("posT", [64, 2, 32], BF16, es)
        bk = self.next_bank()
        for kv in range(2):
            S.op("pe", [possb, self.identb], [bk], lambda kv=kv: nc.tensor.transpose(bk.ap.bitcast(BF16)[0:64, kv * 32:(kv + 1) * 32], possb.ap[:, kv, :], self.identb.ap[0:32, 0:32]), inc=(kv == 1))
        S.op("dve", [bk], [posT], lambda: nc.vector.tensor_copy(posT.ap, bk.ap.bitcast(BF16)[0:64, 0:64].rearrange("p (a c) -> p a c", a=2)))
        posb = self.sbt("posb", [64, 2], F32, es)
        bk2 = self.next_bank()
        for kv in range(2):
            for j in range(32):
                S.op("pe", [w1sb, posT], [bk2], lambda kv=kv, j=j: nc.tensor.matmul(bk2.ap[0:64, kv:kv + 1], w1sb.ap[0:64, kv, j, :], posT.ap[:, kv, j:j + 1], start=(j == 0), stop=(j == 31)), inc=(kv == 1 and j == 31))
        S.op("dve", [bk2], [posb], lambda: nc.vector.tensor_copy(posb.ap, bk2.ap[0:64, 0:2]))
        hid = [self.sbt(f"chid{i}", [64, 128], BF16, es) for i in range(2)]
        hi = 0
        for g in range(2):
            gs = slice(g * 64, (g + 1) * 64)
            for kv in range(2):
                src_t = kT_b[0] if kv == 0 else v0T
                src = k0T.ap if kv == 0 else v0T.ap
                hp = self.next_bank()
                for j in range(32):
                    S.op("pe", [w1sb, src_t], [hp], lambda j=j, hp=hp, kv=kv, src=src: nc.tensor.matmul(hp.ap[0:64, 0:NCMP], w1sb.ap[gs, kv, j, :], src[gs, j:j + 16 * (NCMP - 1) + 1:16], start=(j == 0), stop=(j == 31)), inc=(j == 31))
                hd = hid[hi % 2]
                hi += 1
                S.op("act", [hp, posb], [hd], lambda hp=hp, hd=hd, kv=kv: nc.scalar.activation(out=hd.ap[:, 0:NCMP], in_=hp.ap[0:64, 0:NCMP], func=AF.Gelu_apprx_tanh, bias=posb.ap[:, kv:kv + 1], scale=1.0))
                op_ = self.next_bank()
                if kv == 0:
                    S.op("pe", [hd, w2sb], [op_], lambda hd=hd, op_=op_: nc.tensor.matmul(op_.ap[:, 0:NCMP], w2sb.ap[:, 0:2, :].rearrange("p a c -> p (a c)"), hd.ap[:, 0:NCMP], start=True, stop=True))
                    S.op("dve", [op_], [kcT2[g]], lambda op_=op_, g=g: nc.vector.tensor_copy(kcT2[g].ap[:, 0:NCMP], op_.ap[:, 0:NCMP]))
                else:
                    S.op("pe", [hd, w2sb], [op_], lambda hd=hd, op_=op_: nc.tensor.matmul(op_.ap[0:NCMP, 0:64], hd.ap[:, 0:NCMP], w2sb.ap[:, 2, :], start=True, stop=True))
                    S.op("dve", [op_], [vca[g]], lambda op_=op_, g=g: nc.vector.tensor_copy(vca[g].ap[0:NCMP, 0:64], op_.ap[0:NCMP, 0:64]))

    def nsa_attend(self, l, es, qT, qT_g, kT, kT_b, Va, Va_t, gates, kcT2, vca, selT):
        S, nc = self.S, self.nc
        cmpm = self.sbt("cmpm", [128, SEQ], BF16, es, dma=True)
        S.dma("pool", cmpm, self.c["cmpmask"], cmpm.ap, self.c["cmpmask"].ap)
        pbuf = [self.sbt(f"pbuf{i}", [128, 512], BF16, es) for i in range(3)]
        es_outer = es
        es = es_outer.enter_context(ExitStack())
        frc = self.sbt("force", [128, 2, NT, 32], BF16, es, dma=True)
        S.dma("pool", frc, self.c["force"], frc.ap, self.c["force"].ap)
        prr = [0]
        sbanks = self.bank[0:2]
        srr = [0]
        obanks = self.bank[2:8]

        def next_s():
            b = sbanks[srr[0] % 2]
            srr[0] += 1
            return b

        def next_p():
            p = pbuf[prr[0] % 3]
            prr[0] += 1
            return p

        self.mark("pass1")
        impacc = self.sbt("impacc", [128, NT, 32], F32, es)
        impf = impacc
        rep = self.sbt("rep", [128, NT, 32], F32, es)
        rep1 = self.sbt("rep1", [128, 32], F32, es)
        m8 = self.sbt("m8", [128, 8], F32, es)
        selb = self.sbt("selb", [128, NT, 32], BF16, es)
        rden = self.sbt("rden", [128, 4], F32, es)
        itmp = self.sbt("itmp", [128, 4, 32], F32, es)
        orr = 0
        for g in range(2):
            gs = slice(g * 64, (g + 1) * 64)
            for h in range(3):
                for qg in range(4):
                    qs = slice(qg * 512, (qg + 1) * 512)
                    sbk = next_s()
                    S.op("pe", [kcT2[g], qT_g[qg]], [sbk], lambda sbk=sbk, h=h, qs=qs: nc.tensor.matmul(sbk.ap[0:NCMP, :], kcT2[g].ap[gs, 0:NCMP], qT[gs, h, qs], start=True, stop=True))
                    pc = next_p()
                    S.op("act", [sbk], [pc], lambda sbk=sbk, pc=pc: nc.scalar.activation(out=pc.ap[0:NCMP, :], in_=sbk.ap[0:NCMP, :], func=AF.Exp))
                    S.op("dve", [pc, cmpm], [pc], lambda pc=pc, qs=qs: nc.vector.tensor_tensor(out=pc.ap[0:NCMP, :], in0=pc.ap[0:NCMP, :], in1=cmpm.ap[0:NCMP, qs], op=ALU.mult))
                    ib = obanks[orr % 6]
                    orr += 1
                    for qt in range(4):
                        S.op("pe", [pc, vca[g]], [ib], lambda qt=qt, ib=ib, pc=pc: nc.tensor.matmul(ib.ap[:, qt * 33:(qt + 1) * 33], pc.ap[0:NCMP, qt * 128:(qt + 1) * 128], vca[g].ap[0:NCMP, 64:97], start=True, stop=True), inc=(qt == 3))
                    ibv = ib.ap[:, 0:132].rearrange("p (a c) -> p a c", a=4)
                    S.op("dve", [ib], [rden], lambda ibv=ibv: nc.vector.tensor_scalar_max(rden.ap, ibv[:, :, 0], 1e-30))
                    S.op("dve", [rden], [rden], lambda: nc.vector.reciprocal(rden.ap, rden.ap))
                    acc = impacc.ap[:, 4 * qg:4 * qg + 4, :]
                    rb = rden.ap.unsqueeze(2).to_broadcast([128, 4, 32])
                    if h == 0:
                        S.op("dve", [ib, rden], [impacc], lambda ibv=ibv, acc=acc, rb=rb: nc.vector.tensor_tensor(out=acc, in0=ibv[:, :, 1:33], in1=rb, op=ALU.mult))
                    else:
                        S.op("dve", [ib, rden], [itmp], lambda ibv=ibv, rb=rb: nc.vector.tensor_tensor(out=itmp.ap, in0=ibv[:, :, 1:33], in1=rb, op=ALU.mult))
                        S.op("dve", [itmp, impacc], [impacc], lambda acc=acc: nc.vector.tensor_tensor(out=acc, in0=acc, in1=itmp.ap, op=ALU.add))
            self.mark(f"topk{g}")
            S.op("dve", [impacc, frc], [impf], lambda: nc.vector.tensor_tensor(out=impf.ap, in0=impacc.ap, in1=frc.ap[:, 0], op=ALU.mult))
            S.op("dve", [impf, frc], [impf], lambda: nc.vector.tensor_tensor(out=impf.ap, in0=impf.ap, in1=frc.ap[:, 1], op=ALU.add))
            for t in range(NT):
                S.op("dve", [impf], [m8], lambda t=t: nc.vector.max(out=m8.ap, in_=impf.ap[:, t, :]))
                S.op("dve", [impf, m8], [rep1], lambda t=t: nc.vector.match_replace(out=rep1.ap, in_to_replace=m8.ap, in_values=impf.ap[:, t, :], imm_value=-1e9))
                S.op("dve", [rep1], [m8], lambda: nc.vector.max(out=m8.ap, in_=rep1.ap))
                S.op("dve", [rep1, m8], [rep], lambda t=t: nc.vector.match_replace(out=rep.ap[:, t, :], in_to_replace=m8.ap, in_values=rep1.ap, imm_value=-1e9))
            S.op("dve", [impf, rep], [rep], lambda: nc.vector.tensor_tensor(out=rep.ap, in0=impf.ap, in1=rep.ap, op=ALU.subtract))
            S.op("dve", [rep], [rep], lambda: nc.vector.tensor_scalar(rep.ap, rep.ap, 1.0, -NEG, op0=ALU.min, op1=ALU.mult))
            S.op("dve", [rep], [selb], lambda: nc.vector.tensor_scalar_add(selb.ap, rep.ap, NEG))
            for half in range(2):
                tb_ = next_s()
                pb = tb_.ap.bitcast(BF16)
                for tt in range(8):
                    t = half * 8 + tt
                    S.op("pe", [selb, self.identb], [tb_], lambda t=t, tt=tt, pb=pb, g=g: nc.tensor.transpose(pb[g * 32:(g + 1) * 32, tt * 128:(tt + 1) * 128], selb.ap[:, t, :], self.identb.ap), inc=(tt == 7))
                S.op("act", [tb_], [selT[g]], lambda half=half, pb=pb, g=g: nc.scalar.copy(selT[g].ap[:, half * 1024:(half + 1) * 1024], pb[g * 32:(g + 1) * 32, :]))
        S.barrier()
        es.close()
        es = es_outer
        self.mark("pass2")
        tri = self.sbt("tri", [128, 2, 128], BF16, es, dma=True)
        S.dma("pool", tri, self.c["tri"], tri.ap, self.c["tri"].ap)
        efull = self.sbt("efull", [64, SEQ], BF16, es, dma=True)
        for g in range(2):
            S.dma("pool", efull, self.c["efull"], efull.ap[g * 32:(g + 1) * 32, :], self.c["efull"].ap)
        acc = self.sbt("oacc", [128, 4, 64], F32, es)
        otmp = self.sbt("otmp", [128, 4, 64], F32, es)
        ocomb = [self.sbt(f"ocomb{i}", [128, 4, 64], BF16, es) for i in range(2)]
        den = self.sbt("oden", [128, 3, 4], F32, es)
        oset = 0
        for g in range(2):
            gs = slice(g * 64, (g + 1) * 64)
            for h in range(3):
                hh = g * 3 + h
                chunk, half = hh // 2, hh % 2
                for qg in range(4):
                    qs = slice(qg * 512, (qg + 1) * 512)
                    Ob = obanks[(oset % 2) * 3:(oset % 2) * 3 + 3]
                    oset += 1
                    ofresh = [True, True, True]
                    jobs = [("cmp", 0)] + [("slc", kt) for kt in range(0, 4 * qg + 4)] + [("win", kt) for kt in range(max(0, 4 * qg - 4), 4 * qg + 4)]
                    pend = None
                    for kind, kt in jobs:
                        sbk = next_s()
                        pt = next_p()
                        ks = slice(kt * 128, (kt + 1) * 128)
                        if kind == "cmp":
                            S.op("pe", [kcT2[g], qT_g[qg]], [sbk], lambda sbk=sbk: nc.tensor.matmul(sbk.ap[0:NCMP, :], kcT2[g].ap[gs, 0:NCMP], qT[gs, h, qs], start=True, stop=True))
                            S.op("act", [sbk], [pt], lambda sbk=sbk, pt=pt: nc.scalar.activation(out=pt.ap[0:NCMP, :], in_=sbk.ap[0:NCMP, :], func=AF.Exp))
                            S.op("dve", [pt, cmpm], [pt], lambda pt=pt: nc.vector.tensor_tensor(out=pt.ap[0:NCMP, :], in0=pt.ap[0:NCMP, :], in1=cmpm.ap[0:NCMP, qs], op=ALU.mult))

                            def pv(pt=pt):
                                for qt in range(4):
                                    S.op("pe", [pt, vca[g]], [Ob[0]], lambda qt=qt: nc.tensor.matmul(Ob[0].ap[:, qt * 65:(qt + 1) * 65], pt.ap[0:NCMP, qt * 128:(qt + 1) * 128], vca[g].ap[0:NCMP, 0:65], start=(qt == 0), stop=True), inc=(qt == 3))
                        else:
                            br = 1 if kind == "slc" else 2
                            qlo = max(4 * qg, kt)
                            qhi = 4 * qg + 4 if kind == "slc" else min(kt + 4, 4 * qg + 3) + 1
                            ncols = (qhi - qlo) * 128
                            qsl = slice(qlo * 128, qhi * 128)
                            mms = [(kT[gs, br - 1, ks], qT[gs, h, qsl], 0, ncols, [kT_b[br], qT_g[qg]])]
                            if kind == "slc" and qg >= 2:
                                mms.append((efull.ap[g * 32:(g + 1) * 32, ks], selT[g].ap[:, qsl], 0, ncols, [efull, selT[g]]))
                            if kt >= 4 * qg:
                                mms.append((self.identb.ap, tri.ap[:, 0, :], 0, 128, [self.identb, tri]))
                            if kind == "win" and 4 * qg <= kt + 4 <= 4 * qg + 3:
                                c0 = (kt + 4 - qlo) * 128
                                mms.append((self.identb.ap, tri.ap[:, 1, :], c0, 128, [self.identb, tri]))
                            for i, (lt, rh, c0, nn_, rds) in enumerate(mms):
                                S.op("pe", rds, [sbk], lambda lt=lt, rh=rh, c0=c0, nn_=nn_, i=i, sbk=sbk, n_=len(mms): nc.tensor.matmul(sbk.ap[:, c0:c0 + nn_], lt, rh, start=(i == 0), stop=(i == n_ - 1)), inc=(i == len(mms) - 1))
                            S.op("act", [sbk], [pt], lambda sbk=sbk, pt=pt, ncols=ncols: nc.scalar.activation(out=pt.ap[:, 0:ncols], in_=sbk.ap[:, 0:ncols], func=AF.Exp))
                            ob = Ob[br]
                            vb = br - 1

                            def pv(pt=pt, kind=kind, kt=kt, qlo=qlo, qhi=qhi, ob=ob, vb=vb, br=br, ofresh=ofresh):
                                for qt in range(qlo, qhi):
                                    first = ofresh[br]
                                    ofresh[br] = False
                                    ql = qt - 4 * qg
                                    S.op("pe", [pt, Va_t[kt]], [ob], lambda qt=qt, ql=ql, first=first: nc.tensor.matmul(ob.ap[:, ql * 65:(ql + 1) * 65], pt.ap[:, (qt - qlo) * 128:(qt - qlo + 1) * 128], Va[:, kt * 4 + vb * 2 + g, :], start=first, stop=(kt == qt)), inc=(qt == qhi - 1))
                        if pend is not None:
                            pend()
                        pend = pv
                    pend()
                    oc = ocomb[oset % 2]
                    for b in range(3):
                        obv = Ob[b].ap[:, 0:260].rearrange("p (a c) -> p a c", a=4)
                        S.op("dve", [Ob[b]], [den], lambda obv=obv, b=b: nc.vector.tensor_scalar_max(den.ap[:, b, :], obv[:, :, 64], 1e-30))
                        S.op("dve", [den], [den], lambda b=b: nc.vector.reciprocal(den.ap[:, b, :], den.ap[:, b, :]))
                        S.op("dve", [den, gates], [den], lambda b=b: nc.vector.tensor_tensor(out=den.ap[:, b, :], in0=den.ap[:, b, :], in1=gates.ap[:, 4 * qg:4 * qg + 4, g * 9 + h * 3 + b], op=ALU.mult))
                        cb = den.ap[:, b, :].unsqueeze(2).to_broadcast([128, 4, 64])
                        if b == 0:
                            S.op("dve", [Ob[b], den], [acc], lambda obv=obv, cb=cb: nc.vector.tensor_tensor(out=acc.ap, in0=obv[:, :, 0:64], in1=cb, op=ALU.mult))
                        else:
                            S.op("dve", [Ob[b], den], [otmp], lambda obv=obv, cb=cb: nc.vector.tensor_tensor(out=otmp.ap, in0=obv[:, :, 0:64], in1=cb, op=ALU.mult))
                            dst = acc if b == 1 else oc
                            S.op("dve", [otmp, acc], [dst], lambda dst=dst: nc.vector.tensor_tensor(out=dst.ap, in0=acc.ap, in1=otmp.ap, op=ALU.add))
                    tb_ = next_s()
                    pb = tb_.ap.bitcast(BF16)
                    hs = slice(half * 64, (half + 1) * 64)
                    for qt in range(4):
                        S.op("pe", [oc, self.identb], [tb_], lambda qt=qt, pb=pb, oc=oc: nc.tensor.transpose(pb[hs, qt * 128:(qt + 1) * 128], oc.ap[:, qt, :], self.identb.ap), inc=(qt == 3))
                    S.op("act", [tb_], [self.mT_t[4 * qg + i] for i in range(4)], lambda pb=pb, chunk=chunk: nc.scalar.copy(self.mT[hs, chunk, qs], pb[hs, 0:512]))


_CACHE = {}


def _host_inputs(inputs, n_cores, n_seq):
    consts = _consts()
    maps = []
    x = np.ascontiguousarray(inputs["x"], dtype=np.float32)
    for c in range(n_cores):
        m = {"x": np.ascontiguousarray(x[c * n_seq:(c + 1) * n_seq])}
        for k in WEIGHT_SHAPES:
            m[k] = np.ascontiguousarray(inputs[k], dtype=np.float32)
        for k, v in consts.items():
            m["c_" + k] = np.ascontiguousarray(v, dtype=np.float32)
        maps.append(m)
    return maps


def kernel(**inputs):
    n_cores, n_seq = 8, 2
    b = Builder(n_seq=n_seq)
    nc = b.build()
    maps = _host_inputs(inputs, n_cores, n_seq)
    res = run_bass_kernel_spmd(nc, maps, core_ids=list(range(n_cores)))
    out = np.concatenate([np.asarray(r["out"]) for r in res.results], axis=0)
    return out.astype(np.float32)
```
